# Optimizing a Trainium2 kernel written in Bass

```python
import jax, jax.numpy as jnp
from jax import lax
import numpy as np

D_MODEL = 4096
BATCH = 4
SEQ = 4096
DEPTH = 1

GRID_W = 64
CTX_LEN = 256
EPS = 1e-6

MLSTM_HEADS = 8
MLSTM_WIDTH = D_MODEL // 2
MLSTM_DV = MLSTM_WIDTH // MLSTM_HEADS
MLSTM_DK = MLSTM_DV // 2
CHUNK = 64
CONV_WIDTH = D_MODEL - MLSTM_WIDTH
CONV_K = 3
Q_COLS = MLSTM_HEADS * MLSTM_DK
O_COLS = MLSTM_WIDTH
K_COLS = MLSTM_HEADS * MLSTM_DK
V_COLS = MLSTM_WIDTH
GATE_COLS = 4 * MLSTM_HEADS
CTX_COL0 = Q_COLS + O_COLS + 3 * CONV_WIDTH
IN_COLS = CTX_COL0 + K_COLS + V_COLS + GATE_COLS
N_GROUPS = 4
EXPERTS_PER_GROUP = 4
N_EXPERTS = N_GROUPS * EXPERTS_PER_GROUP
TOP_K_IN_GROUP = 2
D_FF_EXPERT = D_MODEL // 4

kernel_name = "hybrid_mlstm_shortconv_hiermoe_ctxprefix"


def _rms_norm(x, g):
    xf = x.astype(jnp.float32)
    y = xf * lax.rsqrt(jnp.mean(xf * xf, axis=-1, keepdims=True) + EPS)
    return (y * g.astype(jnp.float32)).astype(x.dtype)


def _heads(a, d):
    b, s, _ = a.shape
    return a.reshape(b, s, -1, d).transpose(0, 2, 1, 3)


def _kvg(a, b_gates):
    k = _heads(a[..., :K_COLS], MLSTM_DK).astype(jnp.float32)
    v = _heads(a[..., K_COLS:K_COLS + V_COLS], MLSTM_DV).astype(jnp.float32)
    g = (a[..., K_COLS + V_COLS:] + b_gates).astype(jnp.float32)
    b, s, _ = g.shape
    g = g.reshape(b, s, 4, MLSTM_HEADS).transpose(2, 0, 3, 1)
    g = jnp.stack([g[0], jax.nn.log_sigmoid(g[1]), g[2], jax.nn.log_sigmoid(g[3])])
    return k, v, g


def _to_chunks(a):
    b, h, s = a.shape[:3]
    return jnp.moveaxis(a.reshape(b, h, s // CHUNK, CHUNK, *a.shape[3:]), 2, 0)


def _zero_state(b):
    return (jnp.zeros((b, MLSTM_HEADS, MLSTM_DV, MLSTM_DK), jnp.float32),
            jnp.zeros((b, MLSTM_HEADS, MLSTM_DK), jnp.float32),
            jnp.zeros((b, MLSTM_HEADS), jnp.float32))


def _chunk_state_update(state, k, v, ig, lf):
    C, n, m = state
    bcum = jnp.cumsum(lf, axis=-1)
    g = bcum[..., -1]
    log_w = g[..., None] - bcum + ig
    m_new = jnp.maximum(g + m, jnp.max(log_w, axis=-1))
    w = jnp.exp(log_w - m_new[..., None])
    decay = jnp.exp(g + m - m_new)
    C_new = decay[..., None, None] * C + jnp.einsum("bhs,bhsv,bhsd->bhvd", w, v, k)
    n_new = decay[..., None] * n + jnp.einsum("bhs,bhsd->bhd", w, k)
    return (C_new, n_new, m_new)


def _chunk_output(state, q, k, v, ig, lf):
    C, n, m = state
    bcum = jnp.cumsum(lf, axis=-1)
    tril = jnp.tril(jnp.ones((CHUNK, CHUNK), dtype=bool))
    log_d = jnp.where(tril, bcum[..., :, None] - bcum[..., None, :] + ig[..., None, :], -jnp.inf)
    log_inter = bcum + m[..., None]
    m_t = jnp.maximum(log_inter, jnp.max(log_d, axis=-1))
    d = jnp.exp(log_d - m_t[..., None])
    w_inter = jnp.exp(log_inter - m_t)
    s = jnp.einsum("bhtd,bhsd->bhts", q, k) * d
    num = jnp.einsum("bhts,bhsv->bhtv", s, v) + w_inter[..., None] * jnp.einsum("bhvd,bhtd->bhtv", C, q)
    den = jnp.sum(s, axis=-1) + w_inter * jnp.einsum("bhd,bhtd->bht", n, q)
    return num / jnp.maximum(jnp.abs(den), jnp.exp(-m_t))[..., None]


def _mlstm_context_state(k, v, ig, lf):
    def step(state, xs):
        return _chunk_state_update(state, *xs), None
    state, _ = lax.scan(step, _zero_state(k.shape[0]),
                        (_to_chunks(k), _to_chunks(v), _to_chunks(ig), _to_chunks(lf)))
    return state


def _mlstm_latent(state, q, k, v, ig, lf):
    def step(st, xs):
        qc, kc, vc, igc, lfc = xs
        h = _chunk_output(st, qc, kc, vc, igc, lfc)
        return _chunk_state_update(st, kc, vc, igc, lfc), h
    _, hs = lax.scan(step, state, (_to_chunks(q), _to_chunks(k), _to_chunks(v),
                                   _to_chunks(ig), _to_chunks(lf)))
    b, h, s = q.shape[:3]
    return jnp.moveaxis(hs, 0, 2).reshape(b, h, s, MLSTM_DV)


def _mlstm_direction(q, k, v, ig, lf, ck, cv, cig, clf):
    state = _mlstm_context_state(ck, cv, cig, clf)
    return _mlstm_latent(state, q, k, v, ig, lf)


def _centred_conv3(u, w, axis):
    n = u.shape[axis]
    pad = [(0, 0)] * u.ndim
    pad[axis] = (1, 1)
    up = jnp.pad(u, pad)
    y = w[0] * lax.slice_in_dim(up, 0, n, axis=axis)
    for j in range(1, CONV_K):
        y = y + w[j] * lax.slice_in_dim(up, j, j + n, axis=axis)
    return y


def _token_mixers(xm, cm, w_in, b_gates, conv_w, head_norm, rows):
    b, s, _ = xm.shape
    proj = jnp.dot(xm, w_in)
    o1 = Q_COLS
    o2 = o1 + O_COLS
    o3 = o2 + CONV_WIDTH
    o4 = o3 + CONV_WIDTH
    q = _heads(proj[..., :o1], MLSTM_DK).astype(jnp.float32) * (MLSTM_DK ** -0.5)
    o_gate = proj[..., o1:o2]
    cb = proj[..., o2:o3]
    cc = proj[..., o3:o4]
    cx = proj[..., o4:CTX_COL0]
    k, v, g = _kvg(proj[..., CTX_COL0:], b_gates)
    ck, cv, cg = _kvg(jnp.dot(cm, w_in[:, CTX_COL0:]), b_gates)

    h_fwd = _mlstm_direction(q, k, v, g[0], g[1], ck, cv, cg[0], cg[1])
    flp = lambda a: jnp.flip(a, axis=2)
    h_bwd = flp(_mlstm_direction(flp(q), flp(k), flp(v), flp(g[2]), flp(g[3]),
                                 flp(ck), flp(cv), flp(cg[2]), flp(cg[3])))
    h = h_fwd + h_bwd
    h = h * lax.rsqrt(jnp.mean(h * h, axis=-1, keepdims=True) + EPS)
    h = h * head_norm.astype(jnp.float32).reshape(MLSTM_HEADS, 1, MLSTM_DV)
    h = h.transpose(0, 2, 1, 3).reshape(b, s, MLSTM_WIDTH).astype(xm.dtype)
    y_a = jax.nn.sigmoid(o_gate) * h

    u = (cc * cx).reshape(b, rows, GRID_W, CONV_WIDTH)
    half = CONV_WIDTH // 2
    y_row = _centred_conv3(u[..., :half], conv_w[:, :half], axis=2)
    y_col = _centred_conv3(u[..., half:], conv_w[:, half:], axis=1)
    y_b = cb * jnp.concatenate([y_row, y_col], axis=-1).reshape(b, s, CONV_WIDTH)

    return jnp.concatenate([y_a, y_b], axis=-1)


def _hier_moe(xf, w_rg, b_rg, w_re, b_re, w_e_in, w_e_out):
    b, s, d = xf.shape
    xt = xf.reshape(b * s, d)
    g_logits = (jnp.dot(xt, w_rg) + b_rg).astype(jnp.float32)
    g_sel = jnp.argmax(g_logits, axis=-1)
    p_sel = jnp.max(jax.nn.softmax(g_logits, axis=-1), axis=-1, keepdims=True)
    e_logits = (jnp.dot(xt, w_re) + b_re).astype(jnp.float32).reshape(-1, N_GROUPS, EXPERTS_PER_GROUP)
    e_grp = jnp.einsum("tge,tg->te", e_logits, jax.nn.one_hot(g_sel, N_GROUPS, dtype=jnp.float32))
    top_v, top_i = lax.top_k(e_grp, TOP_K_IN_GROUP)
    w_top = jax.nn.softmax(top_v, axis=-1) * p_sel
    idx = g_sel[:, None] * EXPERTS_PER_GROUP + top_i
    gates = jnp.einsum("tk,tke->et", w_top,
                       jax.nn.one_hot(idx, N_EXPERTS, dtype=jnp.float32)).astype(xt.dtype)

    def expert(acc, p):
        w1, w2, ge = p
        a, u = jnp.split(jnp.dot(xt, w1), 2, axis=-1)
        return acc + ge[:, None] * jnp.dot(jax.nn.silu(a) * u, w2), None

    y, _ = lax.scan(expert, jnp.zeros_like(xt), (w_e_in, w_e_out, gates))
    return y.reshape(b, s, d)


def setup_inputs(seed: int = 0) -> dict:
    key = jax.random.key(seed)
    ks = jax.random.split(key, 20)
    nrm = jax.random.normal
    f32 = jnp.float32
    D = D_MODEL
    gate_base = jnp.repeat(jnp.array([0.0, 3.0, 0.0, 3.0], f32), MLSTM_HEADS)
    return {
        "x": nrm(ks[0], (BATCH, SEQ, D), f32),
        "c": nrm(ks[1], (BATCH, D), f32),
        "ctx": nrm(ks[2], (BATCH, CTX_LEN, D), f32),
        "c_ctx": nrm(ks[3], (D,), f32),
        "w_ada": nrm(ks[4], (DEPTH, D, 6 * D), f32) * (0.5 * D ** -0.5),
        "b_ada": nrm(ks[5], (DEPTH, 6 * D), f32) * 0.02,
        "norm1": 1.0 + 0.05 * nrm(ks[6], (DEPTH, D), f32),
        "w_in": nrm(ks[7], (DEPTH, D, IN_COLS), f32) * D ** -0.5,
        "b_gates": gate_base + 0.5 * nrm(ks[8], (DEPTH, GATE_COLS), f32),
        "conv_w": nrm(ks[9], (DEPTH, CONV_K, CONV_WIDTH), f32) * CONV_K ** -0.5,
        "head_norm": 1.0 + 0.05 * nrm(ks[10], (DEPTH, MLSTM_WIDTH), f32),
        "w_out": nrm(ks[11], (DEPTH, D, D), f32) * D ** -0.5,
        "norm2": 1.0 + 0.05 * nrm(ks[12], (DEPTH, D), f32),
        "w_router_group": nrm(ks[13], (DEPTH, D, N_GROUPS), f32) * D ** -0.5,
        "b_router_group": nrm(ks[14], (DEPTH, N_GROUPS), f32) * 0.01,
        "w_router_expert": nrm(ks[15], (DEPTH, D, N_EXPERTS), f32) * D ** -0.5,
        "b_router_expert": nrm(ks[16], (DEPTH, N_EXPERTS), f32) * 0.01,
        "w_expert_in": nrm(ks[17], (DEPTH, N_EXPERTS, D, 2 * D_FF_EXPERT), f32) * D ** -0.5,
        "w_expert_out": nrm(ks[18], (DEPTH, N_EXPERTS, D_FF_EXPERT, D), f32) * D_FF_EXPERT ** -0.5,
        "final_norm": 1.0 + 0.05 * nrm(ks[19], (D,), f32),
    }


def reference(x, c, ctx, c_ctx, w_ada, b_ada, norm1, w_in, b_gates, conv_w, head_norm, w_out, norm2,
              w_router_group, b_router_group, w_router_expert, b_router_expert,
              w_expert_in, w_expert_out, final_norm):
    b, s, _ = x.shape
    rows = s // GRID_W
    for l in range(DEPTH):
        ada = jnp.dot(jax.nn.silu(c), w_ada[l]) + b_ada[l]
        shift1, scale1, gate1, shift2, scale2, gate2 = jnp.split(ada, 6, axis=-1)
        ada_ctx = jnp.dot(jax.nn.silu(c_ctx), w_ada[l][:, :2 * D_MODEL]) + b_ada[l][:2 * D_MODEL]
        shift_c, scale_c = jnp.split(ada_ctx, 2)
        xm = _rms_norm(x, norm1[l]) * (1 + scale1[:, None]) + shift1[:, None]
        cm = _rms_norm(ctx, norm1[l]) * (1 + scale_c) + shift_c
        mix = _token_mixers(xm, cm, w_in[l], b_gates[l], conv_w[l], head_norm[l], rows)
        x = x + gate1[:, None] * jnp.dot(mix, w_out[l])
        xf = _rms_norm(x, norm2[l]) * (1 + scale2[:, None]) + shift2[:, None]
        x = x + gate2[:, None] * _hier_moe(xf, w_router_group[l], b_router_group[l],
                                           w_router_expert[l], b_router_expert[l],
                                           w_expert_in[l], w_expert_out[l])
    return _rms_norm(x, final_norm)
```

```python
from contextlib import ExitStack
import numpy as np
import concourse.bass as bass
import concourse.mybir as mybir
from concourse.bass_utils import run_bass_kernel_spmd

_DEBUG = False
_DBG_NAMES = ("mixT_d", "qT_d", "kT_d", "k_d", "v_d", "o_d", "g_d", "vec_d")
F32 = mybir.dt.float32
BF16 = mybir.dt.bfloat16
AF = mybir.ActivationFunctionType
ALU = mybir.AluOpType

D = 4096
NCH = 32
TOWN = 2048
TOC = 2304
TALL = 4352
NT_ALL = 34
EPS = 1e-6
QCOL, OCOL, BCOL, CCOL, XCOL, KCOL, VCOL = 0, 1024, 3072, 5120, 7168, 9216, 10240


class Sem:
    __slots__ = ("h", "cnt")

    def __init__(self, h):
        self.h = h
        self.cnt = 0


class Builder:
    ENG = ("sp", "act", "pool", "dve", "pe")

    def __init__(self, nc, name):
        self.nc = nc
        self.name = name
        self.st = ExitStack()
        self.ops = {e: [] for e in self.ENG}
        self.seen = {e: {} for e in self.ENG}
        self.nsem = 0
        self.prog = {e: self.sem("pg" + e) for e in self.ENG}

    def sem(self, nm):
        self.nsem += 1
        return Sem(self.st.enter_context(self.nc.semaphore(f"{self.name}_{nm}_{self.nsem}")))

    def sb(self, nm, shape, dt):
        return self.st.enter_context(self.nc.sbuf_tensor(f"{self.name}_{nm}", shape, dt))

    def wait(self, eng, tok):
        if tok is None:
            return
        s, v = tok
        if v <= 0 or self.seen[eng].get(id(s), 0) >= v:
            return
        self.seen[eng][id(s)] = v
        self.ops[eng].append(lambda e, h=s.h, v=v: e.wait_ge(h, v))

    def op(self, eng, fn, waits=(), sig=None, k=1):
        for t in waits:
            self.wait(eng, t)
        if sig is None:
            self.ops[eng].append(fn)
            return None
        sig.cnt += k
        self.ops[eng].append(lambda e, fn=fn, h=sig.h, k=k: fn(e).then_inc(h, k))
        return (sig, sig.cnt)

    def c(self, eng, fn, waits=()):
        return self.op(eng, fn, waits, self.prog[eng], 1)

    def dma(self, q, out, in_, sem, waits=(), **kw):
        return self.op(q, lambda e, o=out, i=in_, kw=kw: e.dma_start(out=o, in_=i, **kw), waits, sem, 16)

    def run(self):
        with self.nc.Block() as blk:
            @blk.sync
            def _(e):
                for f in self.ops["sp"]:
                    f(e)

            @blk.scalar
            def _(e):
                for f in self.ops["act"]:
                    f(e)

            @blk.gpsimd
            def _(e):
                for f in self.ops["pool"]:
                    f(e)

            @blk.vector
            def _(e):
                for f in self.ops["dve"]:
                    f(e)

            @blk.tensor
            def _(e):
                for f in self.ops["pe"]:
                    f(e)
        self.st.close()


class DmaRing:
    def __init__(self, B, name, bufs):
        self.B = B
        self.bufs = bufs
        self.n = len(bufs)
        self.sems = [B.sem(f"{name}{i}") for i in range(self.n)]
        self.u = 0
        self.rel = [[] for _ in range(self.n)]

    def load(self, q, dmas_fn, extra=()):
        i = self.u % self.n
        self.u += 1
        waits = list(self.rel[i]) + list(extra)
        self.rel[i] = []
        tok = None
        for (o, a, kw) in dmas_fn(self.bufs[i]):
            tok = self.B.dma(q, o, a, self.sems[i], waits, **kw)
            waits = ()
        return i, tok

    def release(self, i, tok):
        self.rel[i].append(tok)


class StoreRing:
    def __init__(self, B, name, bufs):
        self.B = B
        self.bufs = bufs
        self.n = len(bufs)
        self.sems = [B.sem(f"{name}{i}") for i in range(self.n)]
        self.u = 0
        self.done = [None] * self.n

    def acquire(self):
        i = self.u % self.n
        self.u += 1
        return i, ([self.done[i]] if self.done[i] else [])

    def store(self, i, q, out, in_, waits):
        self.done[i] = self.B.dma(q, out, in_, self.sems[i], waits)
        return self.done[i]

    def drain(self, q="sp"):
        for t in self.done:
            self.B.wait(q, t)


class PsBanks:
    def __init__(self, banks):
        self.banks = list(banks)
        self.u = 0
        self.free = {b: [] for b in self.banks}

    def acquire(self):
        b = self.banks[self.u % len(self.banks)]
        self.u += 1
        w = self.free[b]
        self.free[b] = []
        return b, w

    def release(self, b, tok):
        self.free[b].append(tok)


def make_consts(B):
    ident = B.sb("ident", [128, 128], F32)
    B.c("pool", lambda e: e.memset(ident[:], 0.0))
    t = B.c("pool", lambda e: e.affine_select(out=ident[:], in_=ident[:], pattern=[[-1, 128]],
                                              compare_op=ALU.not_equal, fill=1.0, base=0,
                                              channel_multiplier=1))
    return ident, t


def fill_tiles(B, ps, banks, ident, ident_tok, xring, src_fn, n_tiles, svec, shvec, dst_fn, sqj, small,
               post_tile=None, extra_first=()):
    last = []
    first = True
    for i in range(n_tiles):
        slot, ltok = xring.load("sp", lambda buf, i=i: [(buf[:], src_fn(i), {})],
                                extra=extra_first if first else ())
        xb = xring.bufs[slot]
        par = i % 2
        ss = small[:, par:par + 1]
        sq = small[:, 2 + par:3 + par]
        rs = small[:, 4 + par:5 + par]
        t = B.c("act", lambda e, xb=xb, ss=ss: e.activation(out=sqj, in_=xb[:], func=AF.Square, accum_out=ss),
                [ltok])
        t = B.c("act", lambda e, ss=ss, sq=sq: e.activation(out=sq, in_=ss, func=AF.Sqrt, scale=1.0 / D, bias=EPS),
                [t])
        t = B.c("dve", lambda e, sq=sq, rs=rs: e.reciprocal(out=rs, in_=sq), [t])
        t = B.c("dve", lambda e, xb=xb, rs=rs: e.tensor_scalar(out=xb[:], in0=xb[:], scalar1=rs, scalar2=None,
                                                              op0=ALU.mult), [t])
        ptok = None
        last = []
        for g in range(8):
            b, w = banks.acquire()
            w = list(w) + [t] + ([ident_tok] if first else [])
            first = False
            for kk in range(4):
                cch = g * 4 + kk
                fn = (lambda e, b=b, kk=kk, cch=cch, xb=xb: e.transpose(
                    out=ps[:, b, kk * 128:(kk + 1) * 128], in_=xb[:, cch * 128:(cch + 1) * 128], identity=ident[:]))
                if kk == 3:
                    ptok = B.c("pe", fn, w)
                else:
                    B.op("pe", fn, w)
                w = ()
            eng = "act" if g % 2 == 0 else "dve"
            et = None
            for kk in range(4):
                cch = g * 4 + kk
                src = ps[:, b, kk * 128:(kk + 1) * 128]
                dst = dst_fn(cch, i)
                sc = svec[:, cch:cch + 1]
                sh = shvec[:, cch:cch + 1]
                if eng == "act":
                    fn = lambda e, src=src, dst=dst, sc=sc, sh=sh: e.activation(out=dst, in_=src, func=AF.Identity,
                                                                               scale=sc, bias=sh)
                else:
                    fn = lambda e, src=src, dst=dst, sc=sc, sh=sh: e.tensor_scalar(out=dst, in0=src, scalar1=sc,
                                                                                  scalar2=sh, op0=ALU.mult,
                                                                                  op1=ALU.add)
                if kk == 3:
                    et = B.c(eng, fn, [ptok])
                else:
                    B.op(eng, fn, [ptok])
            banks.release(b, et)
            last.append(et)
        xring.release(slot, ptok)
        if post_tile is not None:
            post_tile(i, last[-2:])
    return last[-2:]


def gemm(B, ps, banks, wring, blocks, pre_waits):
    first = [True]

    def compute(blk, slot, tok):
        wb = wring.bufs[slot]
        lastpe = [None]

        def emit_group(bank_n, n_mm, mm_fn, handler):
            b, w = banks.acquire()
            w = list(w) + [tok] + (list(pre_waits) if first[0] else [])
            first[0] = False
            pt = None
            for j in range(n_mm):
                o, l, r = mm_fn(j, b)
                fn = lambda e, o=o, l=l, r=r, j=j: e.matmul(o, lhsT=l, rhs=r, start=(j == 0), stop=(j == n_mm - 1))
                if j == n_mm - 1:
                    pt = B.c("pe", fn, w)
                else:
                    B.op("pe", fn, w)
                w = ()
            lastpe[0] = pt
            et = handler(b, pt)
            banks.release(b, et)

        for part in blk["parts"]:
            part(wb, emit_group)
        wring.release(slot, lastpe[0])

    pending = None
    for blk in list(blocks) + [None]:
        nxt = None
        if blk is not None:
            slot, tok = wring.load("pool", blk["dmas"])
            nxt = (blk, slot, tok)
        if pending is not None:
            compute(*pending)
        pending = nxt


def build_program():
    nc = bass.Bass("TRN2", target_bir_lowering=False)

    def din(name, shape, dt=F32):
        return nc.dram_tensor(name, shape, dt, kind="ExternalInput").ap()

    def dscr(name, shape, dt):
        return nc.dram_tensor(name, shape, dt, kind=("ExternalOutput" if (_DEBUG and name in _DBG_NAMES) else "Internal")).ap()

    x_own = din("x_own", [TOWN, D])
    x_oc = din("x_oc", [TOC, D])
    cT = din("cT", [128, NCH, 2])
    w_ada = din("w_ada", [D, 6 * D])
    b_ada = din("b_ada", [1, 6 * D])
    normsT = din("normsT", [128, 2, NCH])
    w_in = din("w_in", [D, 12320])
    w_g = din("w_g", [D, 32])
    b_g = din("b_g", [1, 32])
    conv_wT = din("conv_wT", [128, 3, 16])
    head_norm = din("head_norm", [1, 2048])
    w_out = din("w_out", [D, D])
    w_r = din("w_r", [D, 20])
    b_r = din("b_r", [1, 20])
    w_e1 = din("w_e1", [16, D, 2048])
    w_e2 = din("w_e2", [16, 1024, D])
    final_norm = din("final_norm", [1, D])
    out = nc.dram_tensor("out", [TOWN, D], F32, kind="ExternalOutput").ap()

    ada_d = dscr("ada_d", [2, 6 * D], F32)
    qT_d = dscr("qT_d", [1024, TOWN], BF16)
    kT_d = dscr("kT_d", [1024, TOWN], BF16)
    convT_d = dscr("convT_d", [6144, TOWN], BF16)
    ccxb_d = dscr("ccxb_d", [2048, 64], BF16)
    o_d = dscr("o_d", [TOWN, 2048], BF16)
    k_d = dscr("k_d", [TALL, 1024], BF16)
    v_d = dscr("v_d", [TALL, 2048], BF16)
    g_d = dscr("g_d", [TALL, 32], F32)
    mixT_d = dscr("mixT_d", [D, TOWN], BF16)
    x1_d = dscr("x1_d", [TOWN, D], F32)
    vec_d = dscr("vec_d", [6, 128, NCH], F32)

    ps_t = nc.psum_tensor("ps", [128, 8, 512], F32).__enter__()
    ps = ps_t

    def wslice(w2d, c0, ncol):
        return w2d.rearrange("(c p) n -> p c n", p=128)[:, :, c0:c0 + ncol]

    B = Builder(nc, "p0")
    scT = B.sb("scT", [128, NCH, 2], BF16)
    cTs = B.sb("cTs", [128, NCH, 2], F32)
    wbufs = [B.sb(f"w{i}", [128, NCH, 512], BF16) for i in range(2)]
    bbufs = [B.sb(f"bb{i}", [2, 512], F32) for i in range(2)]
    obufs = [B.sb(f"ob{i}", [2, 512], F32) for i in range(2)]
    wring = DmaRing(B, "wr", wbufs)
    bring = DmaRing(B, "br", bbufs)
    oring = StoreRing(B, "or", obufs)
    lsem = B.sem("ld")
    t = B.dma("sp", cTs[:], cT, lsem)
    t = B.c("act", lambda e: e.activation(out=scT[:], in_=cTs[:], func=AF.Silu), [t])
    banks = PsBanks([0, 1])

    def ada_blocks():
        for j in range(48):
            def dmas(buf, j=j):
                return [(buf[:], wslice(w_ada, j * 512, 512), {})]

            def part(wb, emit_group, j=j):
                bslot, btok = bring.load("sp", lambda buf, j=j: [
                    (buf[:], b_ada[:, j * 512:(j + 1) * 512].partition_broadcast(2), {})])
                bb = bring.bufs[bslot]

                def mm(c, b, wb=wb):
                    return ps[0:2, b, 0:512], scT[:, c, :], wb[:, c, :]

                def handler(b, pt, bb=bb, bslot=bslot, btok=btok, j=j):
                    oi, ow = oring.acquire()
                    ob = oring.bufs[oi]
                    et = B.c("dve", lambda e: e.tensor_tensor(out=ob[:], in0=ps[0:2, b, 0:512], in1=bb[:],
                                                             op=ALU.add), [pt, btok] + ow)
                    bring.release(bslot, et)
                    oring.store(oi, "sp", ada_d[:, j * 512:(j + 1) * 512], ob[:], [et])
                    return et

                emit_group(1, NCH, mm, handler)

            yield dict(dmas=dmas, parts=[part])

    gemm(B, ps, banks, wring, list(ada_blocks()), [t])
    oring.drain("sp")
    B.run()

    B = Builder(nc, "p0b")
    raw = B.sb("raw", [128, 6, NCH], F32)
    nrm = B.sb("nrm", [128, 2, NCH], F32)
    vec = B.sb("vec", [128, 6, NCH], F32)
    lsem = B.sem("ld")
    ssem = B.sem("st")
    srcs = [(0, D), (0, 0), (1, D), (1, 0), (0, 4 * D), (0, 3 * D)]
    t = None
    for i, (r, off) in enumerate(srcs):
        src = ada_d[r:r + 1, off:off + D].rearrange("o (c p) -> p (o c)", p=128)
        t = B.dma("sp", raw[:, i, :], src, lsem, allow_slow_non_contiguous=True)
    t = B.dma("sp", nrm[:], normsT, lsem)
    for i, ni in ((0, 0), (2, 0), (4, 1)):
        t2 = B.c("dve", lambda e, i=i, ni=ni: e.scalar_tensor_tensor(out=vec[:, i, :], in0=raw[:, i, :], scalar=1.0,
                                                                      in1=nrm[:, ni, :], op0=ALU.add, op1=ALU.mult),
                 [t])
        t2 = B.c("dve", lambda e, i=i: e.tensor_copy(out=vec[:, i + 1, :], in_=raw[:, i + 1, :]), [t])
    st = None
    for i in range(6):
        st = B.dma("sp", vec_d[i], vec[:, i, :], ssem, [t2])
    B.wait("sp", st)
    B.run()

    B = Builder(nc, "p1")
    ident, ident_tok = make_consts(B)
    vecs = B.sb("vecs", [128, 6, NCH], F32)
    xmT = B.sb("xmT", [128, NCH, 1152], BF16)
    xbufs = [B.sb(f"x{i}", [128, D], F32) for i in range(2)]
    sqj = B.sb("sqj", [128, D], BF16)
    small = B.sb("small", [128, 8], F32)
    wbufs = [B.sb(f"w{i}", [128, NCH, 512], BF16) for i in range(2)]
    obufs = [B.sb(f"ob{i}", [128, 512], BF16) for i in range(4)]
    gbufs = [B.sb(f"gb{i}", [128, 32], F32) for i in range(2)]
    xring = DmaRing(B, "xr", xbufs)
    wring = DmaRing(B, "wr", wbufs)
    oring = StoreRing(B, "or", obufs)
    gring = StoreRing(B, "gr", gbufs)
    lsem = B.sem("ld")
    vtok = None
    for i in range(6):
        vtok = B.dma("sp", vecs[:, i, :], vec_d[i], lsem)
    fbanks = PsBanks([0, 1])
    gbanks = PsBanks([2, 3, 4, 5, 6, 7])
    evc = [0]

    def evac_store(ps_ap, np_, nf, pt, dst, scale=None):
        oi, ow = oring.acquire()
        ob = oring.bufs[oi][0:np_, 0:nf]
        eng = "act" if evc[0] % 2 == 0 else "dve"
        evc[0] += 1
        if eng == "act":
            fn = lambda e: e.activation(out=ob, in_=ps_ap, func=AF.Copy, scale=(1.0 if scale is None else scale))
        else:
            fn = lambda e: e.tensor_scalar(out=ob, in0=ps_ap, scalar1=(1.0 if scale is None else scale),
                                           scalar2=None, op0=ALU.mult)
        et = B.c(eng, fn, [pt] + ow)
        oring.store(oi, "sp", dst, ob, [et])
        return et

    def a_part(ntiles, ncol, woff, dst_fn):
        def part(wb, emit_group):
            for tt in range(ntiles):
                def mm(c, b, tt=tt):
                    return ps[:, b, 0:ncol], xmT[:, c, tt * 128:(tt + 1) * 128], wb[:, c, woff:woff + ncol]

                def handler(b, pt, tt=tt):
                    return evac_store(ps[:, b, 0:ncol], 128, ncol, pt, dst_fn(tt))

                emit_group(1, NCH, mm, handler)
        return part

    def b_part(tok_blocks, ncs, woff, dst_fn, scale=None):
        def part(wb, emit_group):
            for cs in range(ncs):
                for (t0, nt) in tok_blocks:
                    def mm(c, b, cs=cs, t0=t0, nt=nt):
                        return (ps[:, b, 0:nt], wb[:, c, woff + cs * 128:woff + (cs + 1) * 128],
                                xmT[:, c, t0:t0 + nt])

                    def handler(b, pt, cs=cs, t0=t0, nt=nt):
                        return evac_store(ps[:, b, 0:nt], 128, nt, pt, dst_fn(cs, t0, nt), scale)

                    emit_group(1, NCH, mm, handler)
        return part

    def g_part(ntiles, tok0):
        def part(wb, emit_group):
            for tt in range(ntiles):
                def mm(c, b, tt=tt):
                    return ps[:, b, 0:32], xmT[:, c, tt * 128:(tt + 1) * 128], wb[:, c, 0:32]

                def handler(b, pt, tt=tt):
                    gi, gw = gring.acquire()
                    gb = gring.bufs[gi]
                    et = B.c("dve", lambda e: e.tensor_copy(out=gb[:], in_=ps[:, b, 0:32]), [pt] + gw)
                    r0 = tok0 + tt * 128
                    gring.store(gi, "sp", g_d[r0:r0 + 128, :], gb[:], [et])
                    return et

                emit_group(1, NCH, mm, handler)
        return part

    def wdma(c0, ncol):
        return lambda buf: [(buf[:, :, 0:ncol], wslice(w_in, c0, ncol), {})]

    for pi in range(4):
        own = pi < 2
        if own:
            ntl = 8
            src = x_own
            r0 = pi * 1024
            tall0 = 2304 + pi * 1024
            sv, shv = vecs[:, 0, :], vecs[:, 1, :]
            fill_specs = [(src, r0, ntl, sv, shv, 0)]
        else:
            ntl = 9
            p2 = pi - 2
            tall0 = p2 * 1152
            if p2 == 0:
                fill_specs = [(x_oc, 0, 2, vecs[:, 2, :], vecs[:, 3, :], 0),
                              (x_oc, 256, 7, vecs[:, 0, :], vecs[:, 1, :], 2)]
            else:
                fill_specs = [(x_oc, 1152, 9, vecs[:, 0, :], vecs[:, 1, :], 0)]
        ftoks = []
        for (src, rr, n, sv, shv, toff) in fill_specs:
            ftoks = fill_tiles(B, ps, fbanks, ident, ident_tok, xring,
                               lambda i, src=src, rr=rr: src[rr + i * 128: rr + (i + 1) * 128, :], n, sv, shv,
                               lambda c, i, toff=toff: xmT[:, c, (toff + i) * 128:(toff + i + 1) * 128],
                               sqj[:], small, extra_first=[vtok]) + ftoks
        blocks = []
        ntok = ntl * 128
        tb = [(0, 512), (512, 512)] + ([(1024, 128)] if ntl == 9 else [])
        if own:
            lo = pi * 1024
            for j in range(2):
                blocks.append(dict(dmas=wdma(QCOL + j * 512, 512), parts=[
                    b_part(tb, 4, 0, lambda cs, t0, nt, j=j: qT_d[j * 512 + cs * 128: j * 512 + (cs + 1) * 128,
                                                                   lo + t0: lo + t0 + nt], scale=128.0 ** -0.5)]))
            for j in range(4):
                blocks.append(dict(dmas=wdma(OCOL + j * 512, 512), parts=[
                    a_part(ntl, 512, 0, lambda tt, j=j: o_d[lo + tt * 128: lo + (tt + 1) * 128,
                                                            j * 512:(j + 1) * 512])]))
            for j in range(12):
                blocks.append(dict(dmas=wdma(BCOL + j * 512, 512), parts=[
                    b_part(tb, 4, 0, lambda cs, t0, nt, j=j: convT_d[j * 512 + cs * 128: j * 512 + (cs + 1) * 128,
                                                                      lo + t0: lo + t0 + nt])]))
        for j in range(2):
            parts = [a_part(ntl, 512, 0, lambda tt, j=j: k_d[tall0 + tt * 128: tall0 + (tt + 1) * 128,
                                                             j * 512:(j + 1) * 512])]
            if own:
                parts.append(b_part(tb, 4, 0, lambda cs, t0, nt, j=j: kT_d[j * 512 + cs * 128: j * 512 + (cs + 1) * 128,
                                                                           lo + t0: lo + t0 + nt]))
            blocks.append(dict(dmas=wdma(KCOL + j * 512, 512), parts=parts))
        for j in range(4):
            blocks.append(dict(dmas=wdma(VCOL + j * 512, 512), parts=[
                a_part(ntl, 512, 0, lambda tt, j=j: v_d[tall0 + tt * 128: tall0 + (tt + 1) * 128,
                                                        j * 512:(j + 1) * 512])]))
        blocks.append(dict(dmas=lambda buf: [(buf[:, :, 0:32], wslice(w_g, 0, 32), {})],
                           parts=[g_part(ntl, tall0)]))
        if pi == 3:
            for half, c0 in ((0, CCOL + 1024), (1, XCOL + 1024)):
                for j in range(2):
                    blocks.append(dict(dmas=wdma(c0 + j * 512, 512), parts=[
                        b_part([(1088, 64)], 4, 0,
                               lambda cs, t0, nt, half=half, j=j: ccxb_d[half * 1024 + j * 512 + cs * 128:
                                                                         half * 1024 + j * 512 + (cs + 1) * 128, :])]))
        gemm(B, ps, gbanks, wring, blocks, ftoks)
        xm_free = (B.prog["pe"], B.prog["pe"].cnt)
        for e in ("act", "dve"):
            B.wait(e, xm_free)
    oring.drain("sp")
    gring.drain("sp")
    B.run()

    B = Builder(nc, "p2")
    ident, ident_tok = make_consts(B)
    triP = B.sb("triP", [128, 128], F32)
    triR = B.sb("triR", [128, 128], F32)
    ones = B.sb("ones", [128, 128], F32)
    B.c("pool", lambda e: e.memset(triP[:], 1.0))
    B.c("pool", lambda e: e.affine_select(out=triP[:], in_=triP[:], pattern=[[1, 128]], compare_op=ALU.is_ge,
                                          fill=0.0, base=0, channel_multiplier=-1))
    B.c("pool", lambda e: e.memset(triR[:], 1.0))
    B.c("pool", lambda e: e.affine_select(out=triR[:], in_=triR[:], pattern=[[-1, 128]], compare_op=ALU.is_ge,
                                          fill=0.0, base=0, channel_multiplier=1))
    ctok = B.c("pool", lambda e: e.memset(ones[:], 1.0))
    G = B.sb("G", [128, NT_ALL, 32], F32)
    bg = B.sb("bg", [128, 32], F32)
    LP = B.sb("LP", [128, NT_ALL, 8], F32)
    LR = B.sb("LR", [128, NT_ALL, 8], F32)
    A_ = B.sb("A", [128, NT_ALL, 16], F32)
    wcol = B.sb("wcol", [128, NT_ALL, 16], F32)
    floor = B.sb("floor", [128, NT_ALL, 16], F32)
    eg = B.sb("eg", [128, NT_ALL, 16], F32)
    hn = B.sb("hn", [128, 2048], F32)
    lsem = B.sem("ld")
    t = B.dma("sp", G[:], g_d.rearrange("(j p) n -> p j n", p=128), lsem)
    t = B.dma("sp", bg[:], b_g.partition_broadcast(128), lsem)
    t = B.dma("sp", hn[:], head_norm.partition_broadcast(128), lsem)
    gt = t
    for j in range(NT_ALL):
        gt2 = B.c("dve", lambda e, j=j: e.tensor_tensor(out=G[:, j, :], in0=G[:, j, :], in1=bg[:], op=ALU.add), [gt])
    ta = B.c("act", lambda e: e.activation(out=LP[:], in_=G[:, :, 8:16], func=AF.Exp, scale=-1.0), [gt2])
    ta = B.c("act", lambda e: e.activation(out=LR[:], in_=G[:, :, 24:32], func=AF.Exp, scale=-1.0), [ta])
    ta = B.c("act", lambda e: e.activation(out=LP[:], in_=LP[:], func=AF.Ln, bias=1.0), [ta])
    ta = B.c("act", lambda e: e.activation(out=LR[:], in_=LR[:], func=AF.Ln, bias=1.0), [ta])
    LPf = LP[:].rearrange("p j h -> p (j h)")
    LRf = LR[:].rearrange("p j h -> p (j h)")
    NJ = NT_ALL * 8
    B.op("pe", lambda e: e.matmul(ps[:, 0, 0:NJ], lhsT=triP[:], rhs=LPf, start=True, stop=True),
         [ta, ctok])
    B.op("pe", lambda e: e.matmul(ps[:, 1, 0:NJ], lhsT=triR[:], rhs=LRf, start=True, stop=True))
    B.op("pe", lambda e: e.matmul(ps[:, 2, 0:NJ], lhsT=ones[:], rhs=LPf, start=True, stop=True))
    tp = B.c("pe", lambda e: e.matmul(ps[:, 3, 0:NJ], lhsT=ones[:], rhs=LRf, start=True, stop=True))

    def v3(bank):
        return ps[:, bank, 0:NJ].rearrange("p (j h) -> p j h", h=8)

    td = B.c("dve", lambda e: e.tensor_tensor(out=A_[:, :, 0:8], in0=v3(0), in1=G[:, :, 0:8], op=ALU.add), [tp])
    td = B.c("dve", lambda e: e.tensor_tensor(out=A_[:, :, 8:16], in0=v3(1), in1=G[:, :, 16:24], op=ALU.add), [td])
    ta = B.c("act", lambda e: e.activation(out=wcol[:], in_=A_[:], func=AF.Exp), [td])
    ta = B.c("act", lambda e: e.activation(out=floor[:, :, 0:8], in_=v3(0), func=AF.Exp), [ta])
    ta = B.c("act", lambda e: e.activation(out=floor[:, :, 8:16], in_=v3(1), func=AF.Exp), [ta])
    ta = B.c("act", lambda e: e.activation(out=eg[:, :, 0:8], in_=v3(2), func=AF.Exp, scale=-1.0), [ta])
    gprep = B.c("act", lambda e: e.activation(out=eg[:, :, 8:16], in_=v3(3), func=AF.Exp, scale=-1.0), [ta])

    hb = []
    for i in range(2):
        hb.append(dict(qT=B.sb(f"qT{i}", [128, TOWN], BF16), kT=B.sb(f"kT{i}", [128, TOWN], BF16),
                       k=B.sb(f"k{i}", [128, NT_ALL, 128], BF16), v=B.sb(f"v{i}", [128, NT_ALL, 256], BF16),
                       o=B.sb(f"o{i}", [128, 16, 256], BF16)))
    hring = DmaRing(B, "hr", hb)
    VP = B.sb("VP", [128, NT_ALL, 257], BF16)
    VR = B.sb("VR", [128, 18, 257], BF16)
    KP = B.sb("KP", [128, NT_ALL, 128], BF16)
    KR = B.sb("KR", [128, 18, 128], BF16)
    C32 = [B.sb(f"C32{i}", [128, 257], F32) for i in range(2)]
    Cbf = [B.sb(f"Cbf{i}", [128, 257], BF16) for i in range(2)]
    hbuf = B.sb("hbuf", [128, 16, 256], F32)
    Sm = [B.sb(f"Sm{i}", [128, 128], BF16) for i in range(2)]
    sm8 = B.sb("sm8", [128, 16], F32)
    hs = [B.sb(f"hs{i}", [128, 256], F32) for i in range(2)]
    sig = [B.sb(f"sig{i}", [128, 256], F32) for i in range(2)]
    yb = [B.sb(f"y{i}", [128, 256], F32) for i in range(2)]
    junk = B.sb("junk", [128, 256], F32)
    yT = [B.sb(f"yT{i}", [128, 2, 128], BF16) for i in range(2)]
    yring = StoreRing(B, "yr", yT)
    psO = PsBanks([1, 2])
    psU = PsBanks([3, 4, 7])
    psS = PsBanks([0, 6])
    psT = PsBanks([5])

    def head_dmas(h):
        def f(buf):
            return [
                (buf["qT"][:], qT_d[h * 128:(h + 1) * 128, :], {}),
                (buf["kT"][:], kT_d[h * 128:(h + 1) * 128, :], {}),
                (buf["k"][:], k_d[:, h * 128:(h + 1) * 128].rearrange("(j p) d -> p j d", p=128), {}),
                (buf["v"][:], v_d[:, h * 256:(h + 1) * 256].rearrange("(j p) d -> p j d", p=128), {}),
                (buf["o"][:], o_d[:, h * 256:(h + 1) * 256].rearrange("(j p) d -> p j d", p=128), {}),
            ]
        return f

    for bk in (psO, psU, psS):
        for b in bk.banks:
            bk.release(b, gprep)
    loaded = hring.load("sp", head_dmas(0))
    prev_head_done = []
    par = [0]
    for h in range(8):
        slot, ltok = loaded
        if h + 1 < 8:
            loaded = hring.load("sp", head_dmas(h + 1))
        hbf = hring.bufs[slot]
        qT, kT, kk, vv, oo = hbf["qT"], hbf["kT"], hbf["k"], hbf["v"], hbf["o"]
        pw = [ltok, gprep] + prev_head_done
        B.c("pool", lambda e, vv=vv, h=h: e.tensor_tensor(
            out=VP[:, :, 0:256], in0=vv[:], in1=wcol[:, :, h:h + 1].to_broadcast([128, NT_ALL, 256]), op=ALU.mult), pw)
        B.c("pool", lambda e, h=h: e.tensor_copy(out=VP[:, :, 256:257], in_=wcol[:, :, h:h + 1]))
        B.c("pool", lambda e, kk=kk, h=h: e.tensor_tensor(
            out=KP[:], in0=kk[:], in1=eg[:, :, h:h + 1].to_broadcast([128, NT_ALL, 128]), op=ALU.mult))
        for (d0, s0, n) in ((0, 0, 2), (2, 18, 16)):
            B.c("pool", lambda e, vv=vv, h=h, d0=d0, s0=s0, n=n: e.tensor_tensor(
                out=VR[:, d0:d0 + n, 0:256], in0=vv[:, s0:s0 + n, :],
                in1=wcol[:, s0:s0 + n, 8 + h:9 + h].to_broadcast([128, n, 256]), op=ALU.mult))
            B.c("pool", lambda e, h=h, d0=d0, s0=s0, n=n: e.tensor_copy(
                out=VR[:, d0:d0 + n, 256:257], in_=wcol[:, s0:s0 + n, 8 + h:9 + h]))
            B.c("pool", lambda e, kk=kk, h=h, d0=d0, s0=s0, n=n: e.tensor_tensor(
                out=KR[:, d0:d0 + n, :], in0=kk[:, s0:s0 + n, :],
                in1=eg[:, s0:s0 + n, 8 + h:9 + h].to_broadcast([128, n, 128]), op=ALU.mult))
        B.c("pool", lambda e: e.memset(C32[0][:], 0.0))
        B.c("pool", lambda e: e.memset(C32[1][:], 0.0))
        B.c("pool", lambda e: e.memset(Cbf[0][:], 0.0))
        prep = B.c("pool", lambda e: e.memset(Cbf[1][:], 0.0))

        Psteps = [(j, j, j - 18 if j >= 18 else None) for j in range(NT_ALL)]
        Rorder = [1, 0] + list(range(33, 17, -1))
        Rsteps = [(j, (j if j < 2 else 2 + j - 18), (j - 18 if j >= 18 else None)) for j in Rorder]
        dP = dict(Vd=VP, Kd=KP, tri=triP, gofs=0, c32=C32[0], cbf=Cbf[0], ctokens=[prep], c32tok=prep, role="combine")
        dR = dict(Vd=VR, Kd=KR, tri=triR, gofs=8, c32=C32[1], cbf=Cbf[1], ctokens=[prep], c32tok=prep, role="store")
        hbuf_tok = {}

        def emit_step(d, j, vi, lo, h=h, qT=qT, kT=kT, oo=oo, ltok=ltok, prep=prep, hbuf_tok=hbuf_tok):
            Vd, Kd, tri, gofs, c32, cbf = d["Vd"], d["Kd"], d["tri"], d["gofs"], d["c32"], d["cbf"]
            full = lo is not None
            if full:
                tsl = slice(lo * 128, (lo + 1) * 128)
                bS, wS = psS.acquire()
                tS = B.c("pe", lambda e: e.matmul(ps[:, bS, 0:128], lhsT=kT[:, tsl], rhs=qT[:, tsl],
                                                  start=True, stop=True), wS + [ltok, prep])
                sm = Sm[par[0] % 2]
                par[0] += 1
                tM = B.c("dve", lambda e: e.tensor_tensor(out=sm[:], in0=ps[:, bS, 0:128], in1=tri[:], op=ALU.mult),
                         [tS])
                psS.release(bS, tM)
                bO, wO = psO.acquire()
                B.op("pe", lambda e: e.matmul(ps[:, bO, 0:257], lhsT=sm[:], rhs=Vd[:, vi, :], start=True, stop=False),
                     wO + [tM])
                tO = B.c("pe", lambda e: e.matmul(ps[:, bO, 0:257], lhsT=qT[:, tsl], rhs=cbf[:], start=False,
                                                  stop=True), d["ctokens"])
                c0 = (par[0] % 2) * 4
                dn = sm8[:, c0:c0 + 1]
                rr = sm8[:, c0 + 1:c0 + 2]
                fl = floor[:, j, gofs + h:gofs + h + 1]
                t1 = B.c("dve", lambda e: e.tensor_scalar(out=rr, in0=ps[:, bO, 256:257], scalar1=-1.0, scalar2=None,
                                                          op0=ALU.mult), [tO])
                t1 = B.c("dve", lambda e: e.tensor_tensor(out=dn, in0=rr, in1=ps[:, bO, 256:257], op=ALU.max), [t1])
                t1 = B.c("dve", lambda e: e.tensor_scalar(out=dn, in0=dn, scalar1=fl, scalar2=None, op0=ALU.max), [t1])
                t1 = B.c("dve", lambda e: e.reciprocal(out=rr, in_=dn), [t1])
                if d["role"] == "store":
                    tE = B.c("act", lambda e: e.activation(out=hbuf[:, lo, :], in_=ps[:, bO, 0:256], func=AF.Copy,
                                                           scale=rr), [t1])
                    psO.release(bO, tE)
                    hbuf_tok[lo] = tE
                else:
                    hsb = hs[lo % 2]
                    sgb = sig[lo % 2]
                    ybb = yb[lo % 2]
                    ssq = sm8[:, c0 + 2:c0 + 3]
                    rs2 = sm8[:, c0 + 3:c0 + 4]
                    tE = B.c("dve", lambda e: e.scalar_tensor_tensor(
                        out=hsb[:], in0=ps[:, bO, 0:256], scalar=rr, in1=hbuf[:, lo, :], op0=ALU.mult,
                        op1=ALU.add), [t1, hbuf_tok[lo]])
                    psO.release(bO, tE)
                    tq = B.c("act", lambda e: e.activation(out=junk[:], in_=hsb[:], func=AF.Square, accum_out=ssq),
                             [tE])
                    tq = B.c("act", lambda e: e.activation(out=ssq, in_=ssq, func=AF.Sqrt, scale=1.0 / 256, bias=EPS),
                             [tq])
                    tg = B.c("act", lambda e: e.activation(out=sgb[:], in_=oo[:, lo, :], func=AF.Sigmoid), [tq])
                    tg = B.c("pool", lambda e: e.tensor_tensor(out=sgb[:], in0=sgb[:],
                                                               in1=hn[:, h * 256:(h + 1) * 256], op=ALU.mult), [tg])
                    t2 = B.c("dve", lambda e: e.reciprocal(out=rs2, in_=ssq), [tq])
                    ty = B.c("dve", lambda e: e.scalar_tensor_tensor(out=ybb[:], in0=hsb[:], scalar=rs2, in1=sgb[:],
                                                                     op0=ALU.mult, op1=ALU.mult), [t2, tg])
                    bT, wT = psT.acquire()
                    B.op("pe", lambda e: e.transpose(out=ps[:, bT, 0:128], in_=ybb[:, 0:128], identity=ident[:]),
                         wT + [ty, ident_tok])
                    tT = B.c("pe", lambda e: e.transpose(out=ps[:, bT, 128:256], in_=ybb[:, 128:256],
                                                         identity=ident[:]))
                    yi, yw = yring.acquire()
                    ytb = yring.bufs[yi]
                    tev = B.c("act", lambda e: e.activation(out=ytb[:].rearrange("p a b -> p (a b)"),
                                                            in_=ps[:, bT, 0:256], func=AF.Copy), [tT] + yw)
                    psT.release(bT, tev)
                    dst = mixT_d[h * 256:(h + 1) * 256, lo * 128:(lo + 1) * 128].rearrange("(i p) t -> p i t", p=128)
                    yring.store(yi, "sp", dst, ytb[:], [tev])
            bU, wU = psU.acquire()
            tU = B.c("pe", lambda e: e.matmul(ps[:, bU, 0:257], lhsT=Kd[:, vi, :], rhs=Vd[:, vi, :], start=True,
                                              stop=True), wU + [prep])
            egs = eg[:, j, gofs + h:gofs + h + 1]
            d["c32tok"] = B.c("dve", lambda e: e.scalar_tensor_tensor(
                out=c32[:], in0=c32[:], scalar=egs, in1=ps[:, bU, 0:257], op0=ALU.mult, op1=ALU.add),
                [tU, d["c32tok"]])
            psU.release(bU, d["c32tok"])
            ct = B.c("act", lambda e: e.activation(out=cbf[:], in_=c32[:], func=AF.Copy), [d["c32tok"]])
            d["ctokens"] = [ct]

        for i in range(NT_ALL):
            emit_step(dP, *Psteps[i])
            if i < len(Rsteps):
                emit_step(dR, *Rsteps[i])
        prev_head_done = [(B.prog[e_], B.prog[e_].cnt) for e_ in ("pe", "act", "dve", "pool")]
        for t_ in prev_head_done:
            hring.release(slot, t_)
    yring.drain("sp")
    B.run()

    B = Builder(nc, "p3")
    cw = B.sb("cw", [128, 3, 16], F32)
    cbufs = []
    for i in range(2):
        cbufs.append(dict(cb=B.sb(f"cb{i}", [128, TOWN], BF16), cc=B.sb(f"cc{i}", [128, TOWN], BF16),
                          cx=B.sb(f"cx{i}", [128, TOWN], BF16), bc=B.sb(f"bc{i}", [128, 64], BF16),
                          bx=B.sb(f"bx{i}", [128, 64], BF16)))
    cring = DmaRing(B, "cr", cbufs)
    u = B.sb("u", [128, TOWN], F32)
    acc = B.sb("acc", [128, TOWN], F32)
    ub = B.sb("ub", [128, 64], F32)
    ybufs = [B.sb(f"yb{i}", [128, TOWN], BF16) for i in range(2)]
    yring = StoreRing(B, "yr", ybufs)
    lsem = B.sem("ld")
    cwt = B.dma("sp", cw[:], conv_wT, lsem)

    def conv_dmas(cb):
        def f(buf):
            l = [(buf["cb"][:], convT_d[cb * 128:(cb + 1) * 128, :], {}),
                 (buf["cc"][:], convT_d[2048 + cb * 128:2048 + (cb + 1) * 128, :], {}),
                 (buf["cx"][:], convT_d[4096 + cb * 128:4096 + (cb + 1) * 128, :], {})]
            if cb >= 8:
                l.append((buf["bc"][:], ccxb_d[(cb - 8) * 128:(cb - 7) * 128, :], {}))
                l.append((buf["bx"][:], ccxb_d[1024 + (cb - 8) * 128:1024 + (cb - 7) * 128, :], {}))
            return l
        return f

    loaded = cring.load("sp", conv_dmas(0))
    tprev = None
    for cb in range(16):
        slot, ltok = loaded
        if cb + 1 < 16:
            loaded = cring.load("sp", conv_dmas(cb + 1))
        bf = cring.bufs[slot]
        wA = cw[:, 0, cb:cb + 1]
        w1 = cw[:, 1, cb:cb + 1]
        wB = cw[:, 2, cb:cb + 1]
        t = B.c("dve", lambda e, bf=bf: e.tensor_tensor(out=u[:], in0=bf["cc"][:], in1=bf["cx"][:], op=ALU.mult),
                [ltok, cwt] + ([tprev] if tprev else []))
        t = B.c("dve", lambda e, w1=w1: e.tensor_scalar(out=acc[:], in0=u[:], scalar1=w1, scalar2=None, op0=ALU.mult),
                [t])
        if cb < 8:
            u3 = u[:].rearrange("p (r c) -> p r c", c=64)
            a3 = acc[:].rearrange("p (r c) -> p r c", c=64)
            t = B.c("dve", lambda e, wA=wA, u3=u3, a3=a3: e.scalar_tensor_tensor(
                out=a3[:, :, 1:64], in0=u3[:, :, 0:63], scalar=wA, in1=a3[:, :, 1:64], op0=ALU.mult, op1=ALU.add), [t])
            t = B.c("dve", lambda e, wB=wB, u3=u3, a3=a3: e.scalar_tensor_tensor(
                out=a3[:, :, 0:63], in0=u3[:, :, 1:64], scalar=wB, in1=a3[:, :, 0:63], op0=ALU.mult, op1=ALU.add), [t])
        else:
            t = B.c("dve", lambda e, wA=wA: e.scalar_tensor_tensor(
                out=acc[:, 64:TOWN], in0=u[:, 0:TOWN - 64], scalar=wA, in1=acc[:, 64:TOWN], op0=ALU.mult,
                op1=ALU.add), [t])
            t = B.c("dve", lambda e, wB=wB: e.scalar_tensor_tensor(
                out=acc[:, 0:TOWN - 64], in0=u[:, 64:TOWN], scalar=wB, in1=acc[:, 0:TOWN - 64], op0=ALU.mult,
                op1=ALU.add), [t])
            t = B.c("dve", lambda e, bf=bf: e.tensor_tensor(out=ub[:], in0=bf["bc"][:], in1=bf["bx"][:], op=ALU.mult),
                    [t])
            t = B.c("dve", lambda e, wA=wA: e.scalar_tensor_tensor(
                out=acc[:, 0:64], in0=ub[:], scalar=wA, in1=acc[:, 0:64], op0=ALU.mult, op1=ALU.add), [t])
        yi, yw = yring.acquire()
        ybf = yring.bufs[yi]
        t = B.c("dve", lambda e, bf=bf, ybf=ybf: e.tensor_tensor(out=ybf[:], in0=acc[:], in1=bf["cb"][:], op=ALU.mult),
                [t] + yw)
        tprev = t
        cring.release(slot, t)
        yring.store(yi, "sp", mixT_d[2048 + cb * 128:2048 + (cb + 1) * 128, :], ybf[:], [t])
    yring.drain("sp")
    B.run()

    B = Builder(nc, "p4")
    mixT = B.sb("mixT", [128, NCH, 1024], BF16)
    wbufs = [B.sb(f"w{i}", [128, NCH, 512], BF16) for i in range(2)]
    g1 = B.sb("g1", [128, D], F32)
    xsb = [B.sb(f"xs{i}", [128, 512], F32) for i in range(4)]
    o32 = [B.sb(f"o32{i}", [128, 512], F32) for i in range(4)]
    wring = DmaRing(B, "wr", wbufs)
    xsring = DmaRing(B, "xs", xsb)
    oring = StoreRing(B, "or", o32)
    lsem = B.sem("ld")
    g1t = B.dma("sp", g1[:], ada_d[0:1, 2 * D:3 * D].partition_broadcast(128), lsem)
    gbanks = PsBanks([0, 1, 2, 3, 4, 5, 6, 7])
    msem = B.sem("mix")
    for pi in range(2):
        lo = pi * 1024
        mw = [(B.prog["pe"], B.prog["pe"].cnt)]
        mt = None
        for q4 in range(4):
            mt = B.dma("sp", mixT[:, q4 * 8:(q4 + 1) * 8, :],
                       mixT_d[q4 * 1024:(q4 + 1) * 1024, lo:lo + 1024].rearrange("(c p) t -> p c t", p=128),
                       msem, mw)
        blocks = []
        for j in range(8):
            def part(wb, emit_group, j=j, lo=lo):
                for tt in range(8):
                    def mm(c, b, tt=tt):
                        return ps[:, b, 0:512], mixT[:, c, tt * 128:(tt + 1) * 128], wb[:, c, :]

                    def handler(b, pt, tt=tt):
                        r0 = lo + tt * 128
                        xi, xt = xsring.load("sp", lambda buf: [(buf[:], x_own[r0:r0 + 128, j * 512:(j + 1) * 512],
                                                                 {})])
                        xb = xsring.bufs[xi]
                        oi, ow = oring.acquire()
                        ob = oring.bufs[oi]
                        et = B.c("dve", lambda e: e.tensor_tensor(out=ob[:], in0=ps[:, b, 0:512],
                                                                 in1=g1[:, j * 512:(j + 1) * 512], op=ALU.mult),
                                 [pt, g1t] + ow)
                        e2 = B.c("pool", lambda e: e.tensor_tensor(out=ob[:], in0=ob[:], in1=xb[:], op=ALU.add),
                                 [et, xt])
                        xsring.release(xi, e2)
                        oring.store(oi, "sp", x1_d[r0:r0 + 128, j * 512:(j + 1) * 512], ob[:], [e2])
                        return et

                    emit_group(1, NCH, mm, handler)
            blocks.append(dict(dmas=lambda buf, j=j: [(buf[:], wslice(w_out, j * 512, 512), {})], parts=[part]))
        gemm(B, ps, gbanks, wring, blocks, [mt])
    oring.drain("sp")
    B.run()

    B = Builder(nc, "p5")
    ident, ident_tok = make_consts(B)
    vecs = B.sb("vecs", [128, 2, NCH], F32)
    xfT = B.sb("xfT", [128, NCH, 512], BF16)
    yacc = B.sb("yacc", [128, 4, D], F32)
    xf32 = yacc[:, 0, :].rearrange("p (c t) -> p c t", t=128)
    wbufs = [B.sb(f"w{i}", [128, NCH, 256], BF16) for i in range(2)]
    w2bufs = [B.sb(f"w2{i}", [128, 8, 512], BF16) for i in range(2)]
    actT = [B.sb(f"actT{i}", [128, 8, 512], BF16) for i in range(2)]
    sqj = actT[0][:].rearrange("p a b -> p (a b)")
    bigA = B.sb("bigA", [128, D], F32)
    bigB = B.sb("bigB", [128, D], F32)
    small = B.sb("small", [128, 8], F32)
    wr32 = B.sb("wr32", [128, NCH, 20], F32)
    br = B.sb("br", [128, 20], F32)
    gates = B.sb("gates", [128, 4, 16], F32)
    rt = B.sb("rt", [128, 64], F32)
    sa = [B.sb(f"sa{i}", [128, 512], F32) for i in range(2)]
    xring = DmaRing(B, "xr", [bigA, bigB])
    wring = DmaRing(B, "wr", wbufs)
    w2ring = DmaRing(B, "w2r", w2bufs)
    lsem = B.sem("ld")
    vtok = B.dma("sp", vecs[:, 0, :], vec_d[4], lsem)
    vtok = B.dma("sp", vecs[:, 1, :], vec_d[5], lsem)
    vtok = B.dma("sp", wr32[:], w_r.rearrange("(c p) n -> p c n", p=128), lsem)
    vtok = B.dma("sp", br[:], b_r.partition_broadcast(128), lsem)
    fbanks = PsBanks([0, 1])
    abanks = PsBanks([0, 1, 2, 3])
    ybanks = PsBanks([4, 5, 6, 7])
    osem = B.sem("out")
    bsem = B.sem("bc")
    out_tok = None
    blk_done = []

    for tb in range(4):
        t0 = tb * 512
        rstate = {"ps2_free": None, "final": [], "dve_last": None}

        def post_tile(i, etoks, tb=tb, rstate=rstate):
            tcst = B.c("pool", lambda e, i=i: e.tensor_copy(out=xfT[:, :, i * 128:(i + 1) * 128], in_=xf32),
                       list(etoks))
            w0 = list(etoks) + [vtok] + ([rstate["ps2_free"]] if rstate["ps2_free"] else [])
            tr = None
            for c in range(NCH):
                fn = lambda e, c=c: e.matmul(ps[:, 2, 0:20], lhsT=xf32[:, c, :], rhs=wr32[:, c, :], start=(c == 0),
                                             stop=(c == NCH - 1))
                if c == NCH - 1:
                    tr = B.c("pe", fn)
                else:
                    B.op("pe", fn, w0 if c == 0 else ())
            lg = rt[:, 0:20]

            def dv(fn, w):
                return B.c("dve", fn, w)

            t = dv(lambda e: e.tensor_tensor(out=lg, in0=ps[:, 2, 0:20], in1=br[:], op=ALU.add),
                   [tr, vtok] + ([rstate["dve_last"]] if rstate["dve_last"] else []))
            rstate["ps2_free"] = t
            mg = rt[:, 20:21]
            t = dv(lambda e: e.tensor_reduce(out=mg, in_=rt[:, 0:4], axis=mybir.AxisListType.X, op=ALU.max), [t])
            ohg = rt[:, 21:25]
            t = dv(lambda e: e.tensor_scalar(out=ohg, in0=rt[:, 0:4], scalar1=mg, scalar2=None, op0=ALU.is_equal), [t])
            eg4 = rt[:, 25:29]
            t = dv(lambda e: e.tensor_scalar(out=eg4, in0=rt[:, 0:4], scalar1=mg, scalar2=None, op0=ALU.subtract), [t])
            ta = B.c("act", lambda e: e.activation(out=eg4, in_=eg4, func=AF.Exp), [t])
            sg = rt[:, 29:30]
            t = dv(lambda e: e.tensor_reduce(out=sg, in_=eg4, axis=mybir.AxisListType.X, op=ALU.add), [ta])
            psel = rt[:, 30:31]
            t = dv(lambda e: e.reciprocal(out=psel, in_=sg), [t])
            egr = rt[:, 31:35]
            t = dv(lambda e: e.tensor_scalar(out=egr, in0=rt[:, 4:8], scalar1=rt[:, 21:22], scalar2=None,
                                             op0=ALU.mult), [t])
            for g in range(1, 4):
                t = dv(lambda e, g=g: e.scalar_tensor_tensor(out=egr, in0=rt[:, 4 + 4 * g:8 + 4 * g],
                                                             scalar=rt[:, 21 + g:22 + g], in1=egr, op0=ALU.mult,
                                                             op1=ALU.add), [t])
            m1 = rt[:, 35:36]
            t = dv(lambda e: e.tensor_reduce(out=m1, in_=egr, axis=mybir.AxisListType.X, op=ALU.max), [t])
            oh1 = rt[:, 36:40]
            t = dv(lambda e: e.tensor_scalar(out=oh1, in0=egr, scalar1=m1, scalar2=None, op0=ALU.is_equal), [t])
            msk = rt[:, 40:44]
            t = dv(lambda e: e.scalar_tensor_tensor(out=msk, in0=oh1, scalar=-1e30, in1=egr, op0=ALU.mult,
                                                    op1=ALU.add), [t])
            m2 = rt[:, 44:45]
            t = dv(lambda e: e.tensor_reduce(out=m2, in_=msk, axis=mybir.AxisListType.X, op=ALU.max), [t])
            oh2 = rt[:, 45:49]
            t = dv(lambda e: e.tensor_scalar(out=oh2, in0=msk, scalar1=m2, scalar2=None, op0=ALU.is_equal), [t])
            dd = rt[:, 49:50]
            t = dv(lambda e: e.tensor_tensor(out=dd, in0=m2, in1=m1, op=ALU.subtract), [t])
            ta = B.c("act", lambda e: e.activation(out=dd, in_=dd, func=AF.Exp), [t])
            w1s = rt[:, 50:51]
            t = dv(lambda e: e.tensor_scalar(out=w1s, in0=dd, scalar1=1.0, scalar2=None, op0=ALU.add), [ta])
            t = dv(lambda e: e.reciprocal(out=w1s, in_=w1s), [t])
            w2s = rt[:, 51:52]
            t = dv(lambda e: e.tensor_tensor(out=w2s, in0=dd, in1=w1s, op=ALU.mult), [t])
            t = dv(lambda e: e.tensor_tensor(out=w1s, in0=w1s, in1=psel, op=ALU.mult), [t])
            t = dv(lambda e: e.tensor_tensor(out=w2s, in0=w2s, in1=psel, op=ALU.mult), [t])
            gg = rt[:, 52:56]
            t = dv(lambda e: e.tensor_scalar(out=gg, in0=oh1, scalar1=w1s, scalar2=None, op0=ALU.mult), [t])
            t = dv(lambda e: e.scalar_tensor_tensor(out=gg, in0=oh2, scalar=w2s, in1=gg, op0=ALU.mult, op1=ALU.add),
                   [t])
            for g in range(4):
                t = dv(lambda e, g=g, i=i: e.tensor_scalar(out=gates[:, i, 4 * g:4 * g + 4], in0=gg,
                                                           scalar1=rt[:, 21 + g:22 + g], scalar2=None, op0=ALU.mult),
                       [t])
            rstate["dve_last"] = t
            rstate["final"] = [t, tcst, tr]
            for e_ in ("act", "dve"):
                B.wait(e_, tcst)
                B.wait(e_, tr)

        ftoks = fill_tiles(B, ps, fbanks, ident, ident_tok, xring,
                           lambda i, t0=t0: x1_d[t0 + i * 128:t0 + (i + 1) * 128, :], 4, vecs[:, 0, :], vecs[:, 1, :],
                           lambda c, i: xf32[:, c, :], sqj, small, post_tile=post_tile,
                           extra_first=[vtok] + blk_done)
        fill_done = list(rstate["final"])
        w2pending = None
        acnt = [0]
        sa_free = [None, None]
        for ex in range(16):
            at = actT[ex % 2]
            blocks = []
            act_toks = []
            for i in range(8):
                def part(wb, emit_group, i=i, at=at):
                    res = {}
                    for half in range(2):
                        def mm(c, b, half=half):
                            return ps[:, b, 0:512], wb[:, c, half * 128:(half + 1) * 128], xfT[:, c, :]

                        def handler(b, pt, half=half):
                            if half == 0:
                                si = acnt[0] % 2
                                sab = sa[si]
                                acnt[0] += 1
                                res["sab"] = sab
                                res["si"] = si
                                et = B.c("act", lambda e: e.activation(out=sab[:], in_=ps[:, b, 0:512], func=AF.Silu),
                                         [pt] + ([sa_free[si]] if sa_free[si] else []))
                                res["sat"] = et
                                return et
                            sab = res["sab"]
                            et = B.c("dve", lambda e: e.tensor_tensor(out=at[:, i, :], in0=ps[:, b, 0:512],
                                                                     in1=sab[:], op=ALU.mult), [pt, res["sat"]])
                            act_toks.append(et)
                            sa_free[res["si"]] = et
                            return et

                        emit_group(1, NCH, mm, handler)
                blocks.append(dict(dmas=lambda buf, ex=ex, i=i: [(buf[:], wslice(w_e1[ex], i * 256, 256), {})],
                                   parts=[part]))
            gemm(B, ps, abanks, wring, blocks, fill_done)
            blocks2 = []
            for j in range(8):
                def part2(wb, emit_group, j=j, ex=ex, at=at):
                    for tt in range(4):
                        def mm(c, b, tt=tt):
                            return ps[:, b, 0:512], at[:, c, tt * 128:(tt + 1) * 128], wb[:, c, :]

                        def handler(b, pt, tt=tt):
                            gsc = gates[:, tt, ex:ex + 1]
                            ya = yacc[:, tt, j * 512:(j + 1) * 512]
                            if ex == 0:
                                return B.c("dve", lambda e: e.tensor_scalar(out=ya, in0=ps[:, b, 0:512], scalar1=gsc,
                                                                           scalar2=None, op0=ALU.mult),
                                           [pt] + fill_done)
                            return B.c("dve", lambda e: e.scalar_tensor_tensor(out=ya, in0=ps[:, b, 0:512],
                                                                              scalar=gsc, in1=ya, op0=ALU.mult,
                                                                              op1=ALU.add), [pt])

                        emit_group(1, 8, mm, handler)
                blocks2.append(dict(dmas=lambda buf, ex=ex, j=j: [
                    (buf[:], w_e2[ex][:, j * 512:(j + 1) * 512].rearrange("(c p) n -> p c n", p=128), {})],
                    parts=[part2]))
            gemm(B, ps, ybanks, w2ring, blocks2, [act_toks[-1]])
            at_free = (B.prog["pe"], B.prog["pe"].cnt)
            B.wait("dve", at_free)
        ydone = (B.prog["dve"], B.prog["dve"].cnt)
        pe_done = (B.prog["pe"], B.prog["pe"].cnt)
        g2t = B.dma("sp", bigA[:], ada_d[0:1, 5 * D:6 * D].partition_broadcast(128), bsem, [pe_done, ydone])
        tz = None
        xtoks = []
        zt = []
        for tt in range(4):
            r0 = t0 + tt * 128
            xt_ = B.dma("sp", bigB[:], x1_d[r0:r0 + 128, :], bsem, [pe_done, ydone] + zt[-1:])
            t = B.c("dve", lambda e, tt=tt: e.tensor_tensor(out=yacc[:, tt, :], in0=yacc[:, tt, :], in1=bigA[:],
                                                           op=ALU.mult), [g2t, ydone])
            t = B.c("pool", lambda e, tt=tt: e.tensor_tensor(out=yacc[:, tt, :], in0=yacc[:, tt, :], in1=bigB[:],
                                                            op=ALU.add), [t, xt_])
            zt.append(t)
        fnt = B.dma("sp", bigA[:], final_norm.partition_broadcast(128), bsem, [(B.prog["dve"], B.prog["dve"].cnt)])
        for tt in range(4):
            r0 = t0 + tt * 128
            ss = small[:, 0:1]
            rs = small[:, 1:2]
            t = B.c("act", lambda e, tt=tt: e.activation(out=bigB[:], in_=yacc[:, tt, :], func=AF.Square,
                                                       accum_out=ss), [zt[tt], zt[3]])
            t = B.c("act", lambda e: e.activation(out=ss, in_=ss, func=AF.Sqrt, scale=1.0 / D, bias=EPS), [t])
            t = B.c("dve", lambda e: e.reciprocal(out=rs, in_=ss), [t])
            t = B.c("dve", lambda e, tt=tt: e.scalar_tensor_tensor(out=yacc[:, tt, :], in0=yacc[:, tt, :], scalar=rs,
                                                                  in1=bigA[:], op0=ALU.mult, op1=ALU.mult), [t, fnt])
            B.wait("act", t)
            out_tok = B.dma("sp", out[r0:r0 + 128, :], yacc[:, tt, :], osem, [t])
        blk_done = [out_tok, (B.prog["dve"], B.prog["dve"].cnt), (B.prog["act"], B.prog["act"].cnt),
                    (B.prog["pool"], B.prog["pool"].cnt)]
    B.wait("sp", out_tok)
    B.run()
    return nc


_CACHE = {}


def kernel(x, c, ctx, c_ctx, w_ada, b_ada, norm1, w_in, b_gates, conv_w, head_norm, w_out, norm2,
           w_router_group, b_router_group, w_router_expert, b_router_expert,
           w_expert_in, w_expert_out, final_norm):
    f = np.float32
    x = np.asarray(x, f)
    ctx = np.asarray(ctx, f)
    c = np.asarray(c, f)
    c_ctx = np.asarray(c_ctx, f)
    w_in0 = np.ascontiguousarray(np.asarray(w_in, f)[0])
    w_ada0 = np.ascontiguousarray(np.asarray(w_ada, f)[0])
    b_ada0 = np.ascontiguousarray(np.asarray(b_ada, f)[0]).reshape(1, -1)
    w_out0 = np.ascontiguousarray(np.asarray(w_out, f)[0])
    normsT = np.ascontiguousarray(
        np.stack([np.asarray(norm1, f)[0], np.asarray(norm2, f)[0]]).reshape(2, NCH, 128).transpose(2, 0, 1))
    wg_nat = np.ascontiguousarray(w_in0[:, 12288:12320])
    perm = np.concatenate([np.arange(16, 32), np.arange(0, 16)])
    wg_sw = np.ascontiguousarray(wg_nat[:, perm])
    bg_nat = np.asarray(b_gates, f)[0].reshape(1, 32)
    bg_sw = np.ascontiguousarray(bg_nat[:, perm])
    cw = np.asarray(conv_w, f)[0]
    cw_nat = np.ascontiguousarray(cw.reshape(3, 16, 128).transpose(2, 0, 1))
    cw_sw = np.ascontiguousarray(cw[::-1].reshape(3, 16, 128).transpose(2, 0, 1))
    hn = np.asarray(head_norm, f)[0].reshape(1, 2048)
    w_r = np.ascontiguousarray(np.concatenate([np.asarray(w_router_group, f)[0], np.asarray(w_router_expert, f)[0]], 1))
    b_r = np.concatenate([np.asarray(b_router_group, f)[0], np.asarray(b_router_expert, f)[0]]).reshape(1, 20)
    we1 = np.asarray(w_expert_in, f)[0]
    we1 = np.ascontiguousarray(we1.reshape(16, D, 2, 8, 128).transpose(0, 1, 3, 2, 4).reshape(16, D, 2048))
    we2 = np.ascontiguousarray(np.asarray(w_expert_out, f)[0])
    fn = np.asarray(final_norm, f).reshape(1, D)

    in_maps = []
    for core in range(8):
        b, hf = core // 2, core % 2
        if hf == 1:
            xo = x[b, 2048:]
            oth = x[b, :2048]
            cp = ctx[b]
            wg, bg, cwt = wg_nat, bg_nat, cw_nat
        else:
            xo = x[b, :2048][::-1]
            oth = x[b, 2048:][::-1]
            cp = ctx[b][::-1]
            wg, bg, cwt = wg_sw, bg_sw, cw_sw
        cT = np.ascontiguousarray(np.stack([c[b], c_ctx]).reshape(2, NCH, 128).transpose(2, 1, 0))
        in_maps.append({
            "x_own": np.ascontiguousarray(xo), "x_oc": np.ascontiguousarray(np.concatenate([cp, oth], 0)),
            "cT": cT, "w_ada": w_ada0, "b_ada": b_ada0, "normsT": normsT, "w_in": w_in0, "w_g": wg, "b_g": bg,
            "conv_wT": cwt, "head_norm": hn, "w_out": w_out0, "w_r": w_r, "b_r": b_r, "w_e1": we1, "w_e2": we2,
            "final_norm": fn,
        })
    if "nc" not in _CACHE:
        _CACHE["nc"] = build_program()
    res = run_bass_kernel_spmd(_CACHE["nc"], in_maps, core_ids=list(range(8)))
    if _DEBUG:
        _CACHE["res"] = res.results
    outp = np.empty((4, 4096, D), f)
    for core in range(8):
        b, hf = core // 2, core % 2
        o = res.results[core]["out"]
        if hf == 1:
            outp[b, 2048:] = o
        else:
            outp[b, :2048] = o[::-1]
    return outp
```

```python
from contextlib import ExitStack
import numpy as np
import concourse.bass as bass
import concourse.mybir as mybir
from concourse.bass_utils import run_bass_kernel_spmd

_DEBUG = False
_DBG_NAMES = ("mixT_d", "qT_d", "kT_d", "k_d", "v_d", "o_d", "g_d", "vec_d")
F32 = mybir.dt.float32
BF16 = mybir.dt.bfloat16
AF = mybir.ActivationFunctionType
ALU = mybir.AluOpType

D = 4096
NCH = 32
TOWN = 2048
TOC = 2304
TALL = 4352
NT_ALL = 34
EPS = 1e-6
QCOL, OCOL, BCOL, CCOL, XCOL, KCOL, VCOL = 0, 1024, 3072, 5120, 7168, 9216, 10240


class Sem:
    __slots__ = ("h", "cnt")

    def __init__(self, h):
        self.h = h
        self.cnt = 0


class Builder:
    ENG = ("sp", "act", "pool", "dve", "pe")

    def __init__(self, nc, name):
        self.nc = nc
        self.name = name
        self.st = ExitStack()
        self.ops = {e: [] for e in self.ENG}
        self.seen = {e: {} for e in self.ENG}
        self.nsem = 0
        self.prog = {e: self.sem("pg" + e) for e in self.ENG}

    def sem(self, nm):
        self.nsem += 1
        return Sem(self.st.enter_context(self.nc.semaphore(f"{self.name}_{nm}_{self.nsem}")))

    def sb(self, nm, shape, dt):
        return self.st.enter_context(self.nc.sbuf_tensor(f"{self.name}_{nm}", shape, dt))

    def wait(self, eng, tok):
        if tok is None:
            return
        s, v = tok
        if v <= 0 or self.seen[eng].get(id(s), 0) >= v:
            return
        self.seen[eng][id(s)] = v
        self.ops[eng].append(lambda e, h=s.h, v=v: e.wait_ge(h, v))

    def op(self, eng, fn, waits=(), sig=None, k=1):
        for t in waits:
            self.wait(eng, t)
        if sig is None:
            self.ops[eng].append(fn)
            return None
        sig.cnt += k
        self.ops[eng].append(lambda e, fn=fn, h=sig.h, k=k: fn(e).then_inc(h, k))
        return (sig, sig.cnt)

    def c(self, eng, fn, waits=()):
        return self.op(eng, fn, waits, self.prog[eng], 1)

    def dma(self, q, out, in_, sem, waits=(), **kw):
        return self.op(q, lambda e, o=out, i=in_, kw=kw: e.dma_start(out=o, in_=i, **kw), waits, sem, 16)

    def run(self):
        with self.nc.Block() as blk:
            @blk.sync
            def _(e):
                for f in self.ops["sp"]:
                    f(e)

            @blk.scalar
            def _(e):
                for f in self.ops["act"]:
                    f(e)

            @blk.gpsimd
            def _(e):
                for f in self.ops["pool"]:
                    f(e)

            @blk.vector
            def _(e):
                for f in self.ops["dve"]:
                    f(e)

            @blk.tensor
            def _(e):
                for f in self.ops["pe"]:
                    f(e)
        self.st.close()


class DmaRing:
    def __init__(self, B, name, bufs):
        self.B = B
        self.bufs = bufs
        self.n = len(bufs)
        self.sems = [B.sem(f"{name}{i}") for i in range(self.n)]
        self.u = 0
        self.rel = [[] for _ in range(self.n)]

    def load(self, q, dmas_fn, extra=()):
        i = self.u % self.n
        self.u += 1
        waits = list(self.rel[i]) + list(extra)
        self.rel[i] = []
        tok = None
        for (o, a, kw) in dmas_fn(self.bufs[i]):
            tok = self.B.dma(q, o, a, self.sems[i], waits, **kw)
            waits = ()
        return i, tok

    def release(self, i, tok):
        self.rel[i].append(tok)


class StoreRing:
    def __init__(self, B, name, bufs):
        self.B = B
        self.bufs = bufs
        self.n = len(bufs)
        self.sems = [B.sem(f"{name}{i}") for i in range(self.n)]
        self.u = 0
        self.done = [None] * self.n

    def acquire(self):
        i = self.u % self.n
        self.u += 1
        return i, ([self.done[i]] if self.done[i] else [])

    def store(self, i, q, out, in_, waits):
        self.done[i] = self.B.dma(q, out, in_, self.sems[i], waits)
        return self.done[i]

    def drain(self, q="sp"):
        for t in self.done:
            self.B.wait(q, t)


class PsBanks:
    def __init__(self, banks):
        self.banks = list(banks)
        self.u = 0
        self.free = {b: [] for b in self.banks}

    def acquire(self):
        b = self.banks[self.u % len(self.banks)]
        self.u += 1
        w = self.free[b]
        self.free[b] = []
        return b, w

    def release(self, b, tok):
        self.free[b].append(tok)


def make_consts(B):
    ident = B.sb("ident", [128, 128], F32)
    B.c("pool", lambda e: e.memset(ident[:], 0.0))
    t = B.c("pool", lambda e: e.affine_select(out=ident[:], in_=ident[:], pattern=[[-1, 128]],
                                              compare_op=ALU.not_equal, fill=1.0, base=0,
                                              channel_multiplier=1))
    return ident, t


def fill_tiles(B, ps, banks, ident, ident_tok, xring, src_fn, n_tiles, svec, shvec, dst_fn, sqj, small,
               post_tile=None, extra_first=()):
    last = []
    first = True
    for i in range(n_tiles):
        slot, ltok = xring.load("sp", lambda buf, i=i: [(buf[:], src_fn(i), {})],
                                extra=extra_first if first else ())
        xb = xring.bufs[slot]
        par = i % 2
        ss = small[:, par:par + 1]
        sq = small[:, 2 + par:3 + par]
        rs = small[:, 4 + par:5 + par]
        t = B.c("act", lambda e, xb=xb, ss=ss: e.activation(out=sqj, in_=xb[:], func=AF.Square, accum_out=ss),
                [ltok])
        t = B.c("act", lambda e, ss=ss, sq=sq: e.activation(out=sq, in_=ss, func=AF.Sqrt, scale=1.0 / D, bias=EPS),
                [t])
        t = B.c("dve", lambda e, sq=sq, rs=rs: e.reciprocal(out=rs, in_=sq), [t])
        t = B.c("dve", lambda e, xb=xb, rs=rs: e.tensor_scalar(out=xb[:], in0=xb[:], scalar1=rs, scalar2=None,
                                                              op0=ALU.mult), [t])
        ptok = None
        last = []
        for g in range(8):
            b, w = banks.acquire()
            w = list(w) + [t] + ([ident_tok] if first else [])
            first = False
            for kk in range(4):
                cch = g * 4 + kk
                fn = (lambda e, b=b, kk=kk, cch=cch, xb=xb: e.transpose(
                    out=ps[:, b, kk * 128:(kk + 1) * 128], in_=xb[:, cch * 128:(cch + 1) * 128], identity=ident[:]))
                if kk == 3:
                    ptok = B.c("pe", fn, w)
                else:
                    B.op("pe", fn, w)
                w = ()
            eng = "act" if g % 2 == 0 else "dve"
            et = None
            for kk in range(4):
                cch = g * 4 + kk
                src = ps[:, b, kk * 128:(kk + 1) * 128]
                dst = dst_fn(cch, i)
                sc = svec[:, cch:cch + 1]
                sh = shvec[:, cch:cch + 1]
                if eng == "act":
                    fn = lambda e, src=src, dst=dst, sc=sc, sh=sh: e.activation(out=dst, in_=src, func=AF.Identity,
                                                                               scale=sc, bias=sh)
                else:
                    fn = lambda e, src=src, dst=dst, sc=sc, sh=sh: e.tensor_scalar(out=dst, in0=src, scalar1=sc,
                                                                                  scalar2=sh, op0=ALU.mult,
                                                                                  op1=ALU.add)
                if kk == 3:
                    et = B.c(eng, fn, [ptok])
                else:
                    B.op(eng, fn, [ptok])
            banks.release(b, et)
            last.append(et)
        xring.release(slot, ptok)
        if post_tile is not None:
            post_tile(i, last[-2:])
    return last[-2:]


def gemm(B, ps, banks, wring, blocks, pre_waits):
    first = [True]

    def compute(blk, slot, tok):
        wb = wring.bufs[slot]
        lastpe = [None]

        def emit_group(bank_n, n_mm, mm_fn, handler):
            b, w = banks.acquire()
            w = list(w) + [tok] + (list(pre_waits) if first[0] else [])
            first[0] = False
            pt = None
            for j in range(n_mm):
                o, l, r = mm_fn(j, b)
                fn = lambda e, o=o, l=l, r=r, j=j: e.matmul(o, lhsT=l, rhs=r, start=(j == 0), stop=(j == n_mm - 1))
                if j == n_mm - 1:
                    pt = B.c("pe", fn, w)
                else:
                    B.op("pe", fn, w)
                w = ()
            lastpe[0] = pt
            et = handler(b, pt)
            banks.release(b, et)

        for part in blk["parts"]:
            part(wb, emit_group)
        wring.release(slot, lastpe[0])

    depth = max(1, wring.n - 1)
    pending = []
    for blk in list(blocks):
        slot, tok = wring.load("pool", blk["dmas"])
        pending.append((blk, slot, tok))
        if len(pending) > depth:
            compute(*pending.pop(0))
    while pending:
        compute(*pending.pop(0))


def build_program():
    nc = bass.Bass("TRN2", target_bir_lowering=False)

    def din(name, shape, dt=F32):
        return nc.dram_tensor(name, shape, dt, kind="ExternalInput").ap()

    def dscr(name, shape, dt):
        return nc.dram_tensor(name, shape, dt, kind=("ExternalOutput" if (_DEBUG and name in _DBG_NAMES) else "Internal")).ap()

    x_own = din("x_own", [TOWN, D])
    x_oc = din("x_oc", [TOC, D])
    cT = din("cT", [128, NCH, 2])
    w_ada = din("w_ada", [D, 6 * D])
    b_ada = din("b_ada", [1, 6 * D])
    normsT = din("normsT", [128, 2, NCH])
    w_in = din("w_in", [D, 12320])
    w_g = din("w_g", [D, 32])
    b_g = din("b_g", [1, 32])
    conv_wT = din("conv_wT", [128, 3, 16])
    head_norm = din("head_norm", [1, 2048])
    w_out = din("w_out", [D, D])
    w_r = din("w_r", [D, 20])
    b_r = din("b_r", [1, 20])
    w_e1 = din("w_e1", [16, D, 2048])
    w_e2 = din("w_e2", [16, 1024, D])
    final_norm = din("final_norm", [1, D])
    out = nc.dram_tensor("out", [TOWN, D], F32, kind="ExternalOutput").ap()

    ada_d = dscr("ada_d", [2, 6 * D], F32)
    qT_d = dscr("qT_d", [1024, TOWN], BF16)
    kT_d = dscr("kT_d", [1024, TOWN], BF16)
    convT_d = dscr("convT_d", [6144, TOWN], BF16)
    ccxb_d = dscr("ccxb_d", [2048, 64], BF16)
    o_d = dscr("o_d", [TOWN, 2048], BF16)
    k_d = dscr("k_d", [TALL, 1024], BF16)
    v_d = dscr("v_d", [TALL, 2048], BF16)
    g_d = dscr("g_d", [TALL, 32], F32)
    mixT_d = dscr("mixT_d", [D, TOWN], BF16)
    x1_d = dscr("x1_d", [TOWN, D], F32)
    vec_d = dscr("vec_d", [6, 128, NCH], F32)

    ps_t = nc.psum_tensor("ps", [128, 8, 512], F32).__enter__()
    ps = ps_t

    def wslice(w2d, c0, ncol):
        return w2d.rearrange("(c p) n -> p c n", p=128)[:, :, c0:c0 + ncol]

    B = Builder(nc, "p0")
    scT = B.sb("scT", [128, NCH, 2], BF16)
    cTs = B.sb("cTs", [128, NCH, 2], F32)
    wbufs = [B.sb(f"w{i}", [128, NCH, 512], BF16) for i in range(4)]
    bbufs = [B.sb(f"bb{i}", [2, 512], F32) for i in range(2)]
    obufs = [B.sb(f"ob{i}", [2, 512], F32) for i in range(2)]
    wring = DmaRing(B, "wr", wbufs)
    bring = DmaRing(B, "br", bbufs)
    oring = StoreRing(B, "or", obufs)
    lsem = B.sem("ld")
    t = B.dma("sp", cTs[:], cT, lsem)
    t = B.c("act", lambda e: e.activation(out=scT[:], in_=cTs[:], func=AF.Silu), [t])
    banks = PsBanks([0, 1])

    def ada_blocks():
        for j in range(48):
            def dmas(buf, j=j):
                return [(buf[:], wslice(w_ada, j * 512, 512), {})]

            def part(wb, emit_group, j=j):
                bslot, btok = bring.load("sp", lambda buf, j=j: [
                    (buf[:], b_ada[:, j * 512:(j + 1) * 512].partition_broadcast(2), {})])
                bb = bring.bufs[bslot]

                def mm(c, b, wb=wb):
                    return ps[0:2, b, 0:512], scT[:, c, :], wb[:, c, :]

                def handler(b, pt, bb=bb, bslot=bslot, btok=btok, j=j):
                    oi, ow = oring.acquire()
                    ob = oring.bufs[oi]
                    et = B.c("dve", lambda e: e.tensor_tensor(out=ob[:], in0=ps[0:2, b, 0:512], in1=bb[:],
                                                             op=ALU.add), [pt, btok] + ow)
                    bring.release(bslot, et)
                    oring.store(oi, "sp", ada_d[:, j * 512:(j + 1) * 512], ob[:], [et])
                    return et

                emit_group(1, NCH, mm, handler)

            yield dict(dmas=dmas, parts=[part])

    gemm(B, ps, banks, wring, list(ada_blocks()), [t])
    oring.drain("sp")
    B.run()

    B = Builder(nc, "p0b")
    raw = B.sb("raw", [128, 6, NCH], F32)
    nrm = B.sb("nrm", [128, 2, NCH], F32)
    vec = B.sb("vec", [128, 6, NCH], F32)
    lsem = B.sem("ld")
    ssem = B.sem("st")
    srcs = [(0, D), (0, 0), (1, D), (1, 0), (0, 4 * D), (0, 3 * D)]
    t = None
    for i, (r, off) in enumerate(srcs):
        src = ada_d[r:r + 1, off:off + D].rearrange("o (c p) -> p (o c)", p=128)
        t = B.dma("sp", raw[:, i, :], src, lsem, allow_slow_non_contiguous=True)
    t = B.dma("sp", nrm[:], normsT, lsem)
    for i, ni in ((0, 0), (2, 0), (4, 1)):
        t2 = B.c("dve", lambda e, i=i, ni=ni: e.scalar_tensor_tensor(out=vec[:, i, :], in0=raw[:, i, :], scalar=1.0,
                                                                      in1=nrm[:, ni, :], op0=ALU.add, op1=ALU.mult),
                 [t])
        t2 = B.c("dve", lambda e, i=i: e.tensor_copy(out=vec[:, i + 1, :], in_=raw[:, i + 1, :]), [t])
    st = None
    for i in range(6):
        st = B.dma("sp", vec_d[i], vec[:, i, :], ssem, [t2])
    B.wait("sp", st)
    B.run()

    B = Builder(nc, "p1")
    ident, ident_tok = make_consts(B)
    vecs = B.sb("vecs", [128, 6, NCH], F32)
    xmT = B.sb("xmT", [128, NCH, 1152], BF16)
    xbufs = [B.sb(f"x{i}", [128, D], F32) for i in range(2)]
    sqj = B.sb("sqj", [128, D], BF16)
    small = B.sb("small", [128, 8], F32)
    wbufs = [B.sb(f"w{i}", [128, NCH, 512], BF16) for i in range(2)]
    obufs = [B.sb(f"ob{i}", [128, 512], BF16) for i in range(4)]
    gbufs = [B.sb(f"gb{i}", [128, 32], F32) for i in range(2)]
    xring = DmaRing(B, "xr", xbufs)
    wring = DmaRing(B, "wr", wbufs)
    oring = StoreRing(B, "or", obufs)
    gring = StoreRing(B, "gr", gbufs)
    lsem = B.sem("ld")
    vtok = None
    for i in range(6):
        vtok = B.dma("sp", vecs[:, i, :], vec_d[i], lsem)
    fbanks = PsBanks([0, 1])
    gbanks = PsBanks([2, 3, 4, 5, 6, 7])
    evc = [0]

    def evac_store(ps_ap, np_, nf, pt, dst, scale=None):
        oi, ow = oring.acquire()
        ob = oring.bufs[oi][0:np_, 0:nf]
        eng = "act" if evc[0] % 2 == 0 else "dve"
        evc[0] += 1
        if eng == "act":
            fn = lambda e: e.activation(out=ob, in_=ps_ap, func=AF.Copy, scale=(1.0 if scale is None else scale))
        else:
            fn = lambda e: e.tensor_scalar(out=ob, in0=ps_ap, scalar1=(1.0 if scale is None else scale),
                                           scalar2=None, op0=ALU.mult)
        et = B.c(eng, fn, [pt] + ow)
        oring.store(oi, "sp", dst, ob, [et])
        return et

    def a_part(ntiles, ncol, woff, dst_fn):
        def part(wb, emit_group):
            for tt in range(ntiles):
                def mm(c, b, tt=tt):
                    return ps[:, b, 0:ncol], xmT[:, c, tt * 128:(tt + 1) * 128], wb[:, c, woff:woff + ncol]

                def handler(b, pt, tt=tt):
                    return evac_store(ps[:, b, 0:ncol], 128, ncol, pt, dst_fn(tt))

                emit_group(1, NCH, mm, handler)
        return part

    def b_part(tok_blocks, ncs, woff, dst_fn, scale=None):
        def part(wb, emit_group):
            for cs in range(ncs):
                for (t0, nt) in tok_blocks:
                    def mm(c, b, cs=cs, t0=t0, nt=nt):
                        return (ps[:, b, 0:nt], wb[:, c, woff + cs * 128:woff + (cs + 1) * 128],
                                xmT[:, c, t0:t0 + nt])

                    def handler(b, pt, cs=cs, t0=t0, nt=nt):
                        return evac_store(ps[:, b, 0:nt], 128, nt, pt, dst_fn(cs, t0, nt), scale)

                    emit_group(1, NCH, mm, handler)
        return part

    def g_part(ntiles, tok0):
        def part(wb, emit_group):
            for tt in range(ntiles):
                def mm(c, b, tt=tt):
                    return ps[:, b, 0:32], xmT[:, c, tt * 128:(tt + 1) * 128], wb[:, c, 0:32]

                def handler(b, pt, tt=tt):
                    gi, gw = gring.acquire()
                    gb = gring.bufs[gi]
                    et = B.c("dve", lambda e: e.tensor_copy(out=gb[:], in_=ps[:, b, 0:32]), [pt] + gw)
                    r0 = tok0 + tt * 128
                    gring.store(gi, "sp", g_d[r0:r0 + 128, :], gb[:], [et])
                    return et

                emit_group(1, NCH, mm, handler)
        return part

    def wdma(c0, ncol):
        return lambda buf: [(buf[:, :, 0:ncol], wslice(w_in, c0, ncol), {})]

    for pi in range(4):
        own = pi < 2
        if own:
            ntl = 8
            src = x_own
            r0 = pi * 1024
            tall0 = 2304 + pi * 1024
            sv, shv = vecs[:, 0, :], vecs[:, 1, :]
            fill_specs = [(src, r0, ntl, sv, shv, 0)]
        else:
            ntl = 9
            p2 = pi - 2
            tall0 = p2 * 1152
            if p2 == 0:
                fill_specs = [(x_oc, 0, 2, vecs[:, 2, :], vecs[:, 3, :], 0),
                              (x_oc, 256, 7, vecs[:, 0, :], vecs[:, 1, :], 2)]
            else:
                fill_specs = [(x_oc, 1152, 9, vecs[:, 0, :], vecs[:, 1, :], 0)]
        ftoks = []
        for (src, rr, n, sv, shv, toff) in fill_specs:
            ftoks = fill_tiles(B, ps, fbanks, ident, ident_tok, xring,
                               lambda i, src=src, rr=rr: src[rr + i * 128: rr + (i + 1) * 128, :], n, sv, shv,
                               lambda c, i, toff=toff: xmT[:, c, (toff + i) * 128:(toff + i + 1) * 128],
                               sqj[:], small, extra_first=[vtok]) + ftoks
        blocks = []
        ntok = ntl * 128
        tb = [(0, 512), (512, 512)] + ([(1024, 128)] if ntl == 9 else [])
        if own:
            lo = pi * 1024
            for j in range(2):
                blocks.append(dict(dmas=wdma(QCOL + j * 512, 512), parts=[
                    b_part(tb, 4, 0, lambda cs, t0, nt, j=j: qT_d[j * 512 + cs * 128: j * 512 + (cs + 1) * 128,
                                                                   lo + t0: lo + t0 + nt], scale=128.0 ** -0.5)]))
            for j in range(4):
                blocks.append(dict(dmas=wdma(OCOL + j * 512, 512), parts=[
                    a_part(ntl, 512, 0, lambda tt, j=j: o_d[lo + tt * 128: lo + (tt + 1) * 128,
                                                            j * 512:(j + 1) * 512])]))
            for j in range(12):
                blocks.append(dict(dmas=wdma(BCOL + j * 512, 512), parts=[
                    b_part(tb, 4, 0, lambda cs, t0, nt, j=j: convT_d[j * 512 + cs * 128: j * 512 + (cs + 1) * 128,
                                                                      lo + t0: lo + t0 + nt])]))
        for j in range(2):
            parts = [a_part(ntl, 512, 0, lambda tt, j=j: k_d[tall0 + tt * 128: tall0 + (tt + 1) * 128,
                                                             j * 512:(j + 1) * 512])]
            if own:
                parts.append(b_part(tb, 4, 0, lambda cs, t0, nt, j=j: kT_d[j * 512 + cs * 128: j * 512 + (cs + 1) * 128,
                                                                           lo + t0: lo + t0 + nt]))
            blocks.append(dict(dmas=wdma(KCOL + j * 512, 512), parts=parts))
        for j in range(4):
            blocks.append(dict(dmas=wdma(VCOL + j * 512, 512), parts=[
                a_part(ntl, 512, 0, lambda tt, j=j: v_d[tall0 + tt * 128: tall0 + (tt + 1) * 128,
                                                        j * 512:(j + 1) * 512])]))
        blocks.append(dict(dmas=lambda buf: [(buf[:, :, 0:32], wslice(w_g, 0, 32), {})],
                           parts=[g_part(ntl, tall0)]))
        if pi == 3:
            for half, c0 in ((0, CCOL + 1024), (1, XCOL + 1024)):
                for j in range(2):
                    blocks.append(dict(dmas=wdma(c0 + j * 512, 512), parts=[
                        b_part([(1088, 64)], 4, 0,
                               lambda cs, t0, nt, half=half, j=j: ccxb_d[half * 1024 + j * 512 + cs * 128:
                                                                         half * 1024 + j * 512 + (cs + 1) * 128, :])]))
        gemm(B, ps, gbanks, wring, blocks, ftoks)
        xm_free = (B.prog["pe"], B.prog["pe"].cnt)
        for e in ("act", "dve"):
            B.wait(e, xm_free)
    oring.drain("sp")
    gring.drain("sp")
    B.run()

    B = Builder(nc, "p2")
    ident, ident_tok = make_consts(B)
    triP = B.sb("triP", [128, 128], F32)
    triR = B.sb("triR", [128, 128], F32)
    ones = B.sb("ones", [128, 128], F32)
    B.c("pool", lambda e: e.memset(triP[:], 1.0))
    B.c("pool", lambda e: e.affine_select(out=triP[:], in_=triP[:], pattern=[[1, 128]], compare_op=ALU.is_ge,
                                          fill=0.0, base=0, channel_multiplier=-1))
    B.c("pool", lambda e: e.memset(triR[:], 1.0))
    B.c("pool", lambda e: e.affine_select(out=triR[:], in_=triR[:], pattern=[[-1, 128]], compare_op=ALU.is_ge,
                                          fill=0.0, base=0, channel_multiplier=1))
    ctok = B.c("pool", lambda e: e.memset(ones[:], 1.0))
    G = B.sb("G", [128, NT_ALL, 32], F32)
    bg = B.sb("bg", [128, 32], F32)
    LP = B.sb("LP", [128, NT_ALL, 8], F32)
    LR = B.sb("LR", [128, NT_ALL, 8], F32)
    A_ = B.sb("A", [128, NT_ALL, 16], F32)
    wcol = B.sb("wcol", [128, NT_ALL, 16], F32)
    floor = B.sb("floor", [128, NT_ALL, 16], F32)
    eg = B.sb("eg", [128, NT_ALL, 16], F32)
    hn = B.sb("hn", [128, 2048], F32)
    lsem = B.sem("ld")
    t = B.dma("sp", G[:], g_d.rearrange("(j p) n -> p j n", p=128), lsem)
    t = B.dma("sp", bg[:], b_g.partition_broadcast(128), lsem)
    t = B.dma("sp", hn[:], head_norm.partition_broadcast(128), lsem)
    gt = t
    for j in range(NT_ALL):
        gt2 = B.c("dve", lambda e, j=j: e.tensor_tensor(out=G[:, j, :], in0=G[:, j, :], in1=bg[:], op=ALU.add), [gt])
    ta = B.c("act", lambda e: e.activation(out=LP[:], in_=G[:, :, 8:16], func=AF.Exp, scale=-1.0), [gt2])
    ta = B.c("act", lambda e: e.activation(out=LR[:], in_=G[:, :, 24:32], func=AF.Exp, scale=-1.0), [ta])
    ta = B.c("act", lambda e: e.activation(out=LP[:], in_=LP[:], func=AF.Ln, bias=1.0), [ta])
    ta = B.c("act", lambda e: e.activation(out=LR[:], in_=LR[:], func=AF.Ln, bias=1.0), [ta])
    LPf = LP[:].rearrange("p j h -> p (j h)")
    LRf = LR[:].rearrange("p j h -> p (j h)")
    NJ = NT_ALL * 8
    B.op("pe", lambda e: e.matmul(ps[:, 0, 0:NJ], lhsT=triP[:], rhs=LPf, start=True, stop=True),
         [ta, ctok])
    B.op("pe", lambda e: e.matmul(ps[:, 1, 0:NJ], lhsT=triR[:], rhs=LRf, start=True, stop=True))
    B.op("pe", lambda e: e.matmul(ps[:, 2, 0:NJ], lhsT=ones[:], rhs=LPf, start=True, stop=True))
    tp = B.c("pe", lambda e: e.matmul(ps[:, 3, 0:NJ], lhsT=ones[:], rhs=LRf, start=True, stop=True))

    def v3(bank):
        return ps[:, bank, 0:NJ].rearrange("p (j h) -> p j h", h=8)

    td = B.c("dve", lambda e: e.tensor_tensor(out=A_[:, :, 0:8], in0=v3(0), in1=G[:, :, 0:8], op=ALU.add), [tp])
    td = B.c("dve", lambda e: e.tensor_tensor(out=A_[:, :, 8:16], in0=v3(1), in1=G[:, :, 16:24], op=ALU.add), [td])
    ta = B.c("act", lambda e: e.activation(out=wcol[:], in_=A_[:], func=AF.Exp), [td])
    ta = B.c("act", lambda e: e.activation(out=floor[:, :, 0:8], in_=v3(0), func=AF.Exp), [ta])
    ta = B.c("act", lambda e: e.activation(out=floor[:, :, 8:16], in_=v3(1), func=AF.Exp), [ta])
    ta = B.c("act", lambda e: e.activation(out=eg[:, :, 0:8], in_=v3(2), func=AF.Exp, scale=-1.0), [ta])
    gprep = B.c("act", lambda e: e.activation(out=eg[:, :, 8:16], in_=v3(3), func=AF.Exp, scale=-1.0), [ta])

    hb = []
    for i in range(2):
        hb.append(dict(qT=B.sb(f"qT{i}", [128, TOWN], BF16), kT=B.sb(f"kT{i}", [128, TOWN], BF16),
                       k=B.sb(f"k{i}", [128, NT_ALL, 128], BF16), v=B.sb(f"v{i}", [128, NT_ALL, 256], BF16),
                       o=B.sb(f"o{i}", [128, 16, 256], BF16)))
    hring = DmaRing(B, "hr", hb)
    VP = B.sb("VP", [128, NT_ALL, 257], BF16)
    VR = B.sb("VR", [128, 18, 257], BF16)
    KP = B.sb("KP", [128, NT_ALL, 128], BF16)
    KR = B.sb("KR", [128, 18, 128], BF16)
    C32 = [B.sb(f"C32{i}", [128, 257], F32) for i in range(2)]
    Cbf = [B.sb(f"Cbf{i}", [128, 257], BF16) for i in range(2)]
    hbuf = B.sb("hbuf", [128, 16, 256], F32)
    Sm = [B.sb(f"Sm{i}", [128, 128], BF16) for i in range(2)]
    sm8 = B.sb("sm8", [128, 16], F32)
    hs = [B.sb(f"hs{i}", [128, 256], F32) for i in range(2)]
    sig = [B.sb(f"sig{i}", [128, 256], F32) for i in range(2)]
    yb = [B.sb(f"y{i}", [128, 256], F32) for i in range(2)]
    junk = B.sb("junk", [128, 256], F32)
    yT = [B.sb(f"yT{i}", [128, 2, 128], BF16) for i in range(2)]
    yring = StoreRing(B, "yr", yT)
    psO = PsBanks([1, 2])
    psU = PsBanks([3, 4, 7])
    psS = PsBanks([0, 6])
    psT = PsBanks([5])

    def head_dmas(h):
        def f(buf):
            return [
                (buf["qT"][:], qT_d[h * 128:(h + 1) * 128, :], {}),
                (buf["kT"][:], kT_d[h * 128:(h + 1) * 128, :], {}),
                (buf["k"][:], k_d[:, h * 128:(h + 1) * 128].rearrange("(j p) d -> p j d", p=128), {}),
                (buf["v"][:], v_d[:, h * 256:(h + 1) * 256].rearrange("(j p) d -> p j d", p=128), {}),
                (buf["o"][:], o_d[:, h * 256:(h + 1) * 256].rearrange("(j p) d -> p j d", p=128), {}),
            ]
        return f

    for bk in (psO, psU, psS):
        for b in bk.banks:
            bk.release(b, gprep)
    loaded = hring.load("sp", head_dmas(0))
    prev_head_done = []
    par = [0]
    for h in range(8):
        slot, ltok = loaded
        if h + 1 < 8:
            loaded = hring.load("sp", head_dmas(h + 1))
        hbf = hring.bufs[slot]
        qT, kT, kk, vv, oo = hbf["qT"], hbf["kT"], hbf["k"], hbf["v"], hbf["o"]
        pw = [ltok, gprep] + prev_head_done
        B.c("pool", lambda e, vv=vv, h=h: e.tensor_tensor(
            out=VP[:, :, 0:256], in0=vv[:], in1=wcol[:, :, h:h + 1].to_broadcast([128, NT_ALL, 256]), op=ALU.mult), pw)
        B.c("pool", lambda e, h=h: e.tensor_copy(out=VP[:, :, 256:257], in_=wcol[:, :, h:h + 1]))
        B.c("pool", lambda e, kk=kk, h=h: e.tensor_tensor(
            out=KP[:], in0=kk[:], in1=eg[:, :, h:h + 1].to_broadcast([128, NT_ALL, 128]), op=ALU.mult))
        for (d0, s0, n) in ((0, 0, 2), (2, 18, 16)):
            B.c("pool", lambda e, vv=vv, h=h, d0=d0, s0=s0, n=n: e.tensor_tensor(
                out=VR[:, d0:d0 + n, 0:256], in0=vv[:, s0:s0 + n, :],
                in1=wcol[:, s0:s0 + n, 8 + h:9 + h].to_broadcast([128, n, 256]), op=ALU.mult))
            B.c("pool", lambda e, h=h, d0=d0, s0=s0, n=n: e.tensor_copy(
                out=VR[:, d0:d0 + n, 256:257], in_=wcol[:, s0:s0 + n, 8 + h:9 + h]))
            B.c("pool", lambda e, kk=kk, h=h, d0=d0, s0=s0, n=n: e.tensor_tensor(
                out=KR[:, d0:d0 + n, :], in0=kk[:, s0:s0 + n, :],
                in1=eg[:, s0:s0 + n, 8 + h:9 + h].to_broadcast([128, n, 128]), op=ALU.mult))
        B.c("pool", lambda e: e.memset(C32[0][:], 0.0))
        B.c("pool", lambda e: e.memset(C32[1][:], 0.0))
        B.c("pool", lambda e: e.memset(Cbf[0][:], 0.0))
        prep = B.c("pool", lambda e: e.memset(Cbf[1][:], 0.0))

        Psteps = [(j, j, j - 18 if j >= 18 else None) for j in range(NT_ALL)]
        Rorder = [1, 0] + list(range(33, 17, -1))
        Rsteps = [(j, (j if j < 2 else 2 + j - 18), (j - 18 if j >= 18 else None)) for j in Rorder]
        dP = dict(Vd=VP, Kd=KP, tri=triP, gofs=0, c32=C32[0], cbf=Cbf[0], ctokens=[prep], c32tok=prep, role="combine")
        dR = dict(Vd=VR, Kd=KR, tri=triR, gofs=8, c32=C32[1], cbf=Cbf[1], ctokens=[prep], c32tok=prep, role="store")
        hbuf_tok = {}

        def emit_step(d, j, vi, lo, h=h, qT=qT, kT=kT, oo=oo, ltok=ltok, prep=prep, hbuf_tok=hbuf_tok):
            Vd, Kd, tri, gofs, c32, cbf = d["Vd"], d["Kd"], d["tri"], d["gofs"], d["c32"], d["cbf"]
            full = lo is not None
            if full:
                tsl = slice(lo * 128, (lo + 1) * 128)
                bS, wS = psS.acquire()
                tS = B.c("pe", lambda e: e.matmul(ps[:, bS, 0:128], lhsT=kT[:, tsl], rhs=qT[:, tsl],
                                                  start=True, stop=True), wS + [ltok, prep])
                sm = Sm[par[0] % 2]
                par[0] += 1
                tM = B.c("dve", lambda e: e.tensor_tensor(out=sm[:], in0=ps[:, bS, 0:128], in1=tri[:], op=ALU.mult),
                         [tS])
                psS.release(bS, tM)
                bO, wO = psO.acquire()
                B.op("pe", lambda e: e.matmul(ps[:, bO, 0:257], lhsT=sm[:], rhs=Vd[:, vi, :], start=True, stop=False),
                     wO + [tM])
                tO = B.c("pe", lambda e: e.matmul(ps[:, bO, 0:257], lhsT=qT[:, tsl], rhs=cbf[:], start=False,
                                                  stop=True), d["ctokens"])
                c0 = (par[0] % 2) * 4
                dn = sm8[:, c0:c0 + 1]
                rr = sm8[:, c0 + 1:c0 + 2]
                fl = floor[:, j, gofs + h:gofs + h + 1]
                t1 = B.c("dve", lambda e: e.tensor_scalar(out=rr, in0=ps[:, bO, 256:257], scalar1=-1.0, scalar2=None,
                                                          op0=ALU.mult), [tO])
                t1 = B.c("dve", lambda e: e.tensor_tensor(out=dn, in0=rr, in1=ps[:, bO, 256:257], op=ALU.max), [t1])
                t1 = B.c("dve", lambda e: e.tensor_scalar(out=dn, in0=dn, scalar1=fl, scalar2=None, op0=ALU.max), [t1])
                t1 = B.c("dve", lambda e: e.reciprocal(out=rr, in_=dn), [t1])
                if d["role"] == "store":
                    tE = B.c("act", lambda e: e.activation(out=hbuf[:, lo, :], in_=ps[:, bO, 0:256], func=AF.Copy,
                                                           scale=rr), [t1])
                    psO.release(bO, tE)
                    hbuf_tok[lo] = tE
                else:
                    hsb = hs[lo % 2]
                    sgb = sig[lo % 2]
                    ybb = yb[lo % 2]
                    ssq = sm8[:, c0 + 2:c0 + 3]
                    rs2 = sm8[:, c0 + 3:c0 + 4]
                    tE = B.c("dve", lambda e: e.scalar_tensor_tensor(
                        out=hsb[:], in0=ps[:, bO, 0:256], scalar=rr, in1=hbuf[:, lo, :], op0=ALU.mult,
                        op1=ALU.add), [t1, hbuf_tok[lo]])
                    psO.release(bO, tE)
                    tq = B.c("act", lambda e: e.activation(out=junk[:], in_=hsb[:], func=AF.Square, accum_out=ssq),
                             [tE])
                    tq = B.c("act", lambda e: e.activation(out=ssq, in_=ssq, func=AF.Sqrt, scale=1.0 / 256, bias=EPS),
                             [tq])
                    tg = B.c("act", lambda e: e.activation(out=sgb[:], in_=oo[:, lo, :], func=AF.Sigmoid), [tq])
                    tg = B.c("pool", lambda e: e.tensor_tensor(out=sgb[:], in0=sgb[:],
                                                               in1=hn[:, h * 256:(h + 1) * 256], op=ALU.mult), [tg])
                    t2 = B.c("dve", lambda e: e.reciprocal(out=rs2, in_=ssq), [tq])
                    ty = B.c("dve", lambda e: e.scalar_tensor_tensor(out=ybb[:], in0=hsb[:], scalar=rs2, in1=sgb[:],
                                                                     op0=ALU.mult, op1=ALU.mult), [t2, tg])
                    bT, wT = psT.acquire()
                    B.op("pe", lambda e: e.transpose(out=ps[:, bT, 0:128], in_=ybb[:, 0:128], identity=ident[:]),
                         wT + [ty, ident_tok])
                    tT = B.c("pe", lambda e: e.transpose(out=ps[:, bT, 128:256], in_=ybb[:, 128:256],
                                                         identity=ident[:]))
                    yi, yw = yring.acquire()
                    ytb = yring.bufs[yi]
                    tev = B.c("act", lambda e: e.activation(out=ytb[:].rearrange("p a b -> p (a b)"),
                                                            in_=ps[:, bT, 0:256], func=AF.Copy), [tT] + yw)
                    psT.release(bT, tev)
                    dst = mixT_d[h * 256:(h + 1) * 256, lo * 128:(lo + 1) * 128].rearrange("(i p) t -> p i t", p=128)
                    yring.store(yi, "sp", dst, ytb[:], [tev])
            bU, wU = psU.acquire()
            tU = B.c("pe", lambda e: e.matmul(ps[:, bU, 0:257], lhsT=Kd[:, vi, :], rhs=Vd[:, vi, :], start=True,
                                              stop=True), wU + [prep])
            egs = eg[:, j, gofs + h:gofs + h + 1]
            d["c32tok"] = B.c("dve", lambda e: e.scalar_tensor_tensor(
                out=c32[:], in0=c32[:], scalar=egs, in1=ps[:, bU, 0:257], op0=ALU.mult, op1=ALU.add),
                [tU, d["c32tok"]])
            psU.release(bU, d["c32tok"])
            ct = B.c("act", lambda e: e.activation(out=cbf[:], in_=c32[:], func=AF.Copy), [d["c32tok"]])
            d["ctokens"] = [ct]

        for i in range(NT_ALL):
            emit_step(dP, *Psteps[i])
            if i < len(Rsteps):
                emit_step(dR, *Rsteps[i])
        prev_head_done = [(B.prog[e_], B.prog[e_].cnt) for e_ in ("pe", "act", "dve", "pool")]
        for t_ in prev_head_done:
            hring.release(slot, t_)
    yring.drain("sp")
    B.run()

    B = Builder(nc, "p3")
    cw = B.sb("cw", [128, 3, 16], F32)
    cbufs = []
    for i in range(2):
        cbufs.append(dict(cb=B.sb(f"cb{i}", [128, TOWN], BF16), cc=B.sb(f"cc{i}", [128, TOWN], BF16),
                          cx=B.sb(f"cx{i}", [128, TOWN], BF16), bc=B.sb(f"bc{i}", [128, 64], BF16),
                          bx=B.sb(f"bx{i}", [128, 64], BF16)))
    cring = DmaRing(B, "cr", cbufs)
    u = B.sb("u", [128, TOWN], F32)
    acc = B.sb("acc", [128, TOWN], F32)
    ub = B.sb("ub", [128, 64], F32)
    ybufs = [B.sb(f"yb{i}", [128, TOWN], BF16) for i in range(2)]
    yring = StoreRing(B, "yr", ybufs)
    lsem = B.sem("ld")
    cwt = B.dma("sp", cw[:], conv_wT, lsem)

    def conv_dmas(cb):
        def f(buf):
            l = [(buf["cb"][:], convT_d[cb * 128:(cb + 1) * 128, :], {}),
                 (buf["cc"][:], convT_d[2048 + cb * 128:2048 + (cb + 1) * 128, :], {}),
                 (buf["cx"][:], convT_d[4096 + cb * 128:4096 + (cb + 1) * 128, :], {})]
            if cb >= 8:
                l.append((buf["bc"][:], ccxb_d[(cb - 8) * 128:(cb - 7) * 128, :], {}))
                l.append((buf["bx"][:], ccxb_d[1024 + (cb - 8) * 128:1024 + (cb - 7) * 128, :], {}))
            return l
        return f

    loaded = cring.load("sp", conv_dmas(0))
    tprev = None
    for cb in range(16):
        slot, ltok = loaded
        if cb + 1 < 16:
            loaded = cring.load("sp", conv_dmas(cb + 1))
        bf = cring.bufs[slot]
        wA = cw[:, 0, cb:cb + 1]
        w1 = cw[:, 1, cb:cb + 1]
        wB = cw[:, 2, cb:cb + 1]
        t = B.c("dve", lambda e, bf=bf: e.tensor_tensor(out=u[:], in0=bf["cc"][:], in1=bf["cx"][:], op=ALU.mult),
                [ltok, cwt] + ([tprev] if tprev else []))
        t = B.c("dve", lambda e, w1=w1: e.tensor_scalar(out=acc[:], in0=u[:], scalar1=w1, scalar2=None, op0=ALU.mult),
                [t])
        if cb < 8:
            u3 = u[:].rearrange("p (r c) -> p r c", c=64)
            a3 = acc[:].rearrange("p (r c) -> p r c", c=64)
            t = B.c("dve", lambda e, wA=wA, u3=u3, a3=a3: e.scalar_tensor_tensor(
                out=a3[:, :, 1:64], in0=u3[:, :, 0:63], scalar=wA, in1=a3[:, :, 1:64], op0=ALU.mult, op1=ALU.add), [t])
            t = B.c("dve", lambda e, wB=wB, u3=u3, a3=a3: e.scalar_tensor_tensor(
                out=a3[:, :, 0:63], in0=u3[:, :, 1:64], scalar=wB, in1=a3[:, :, 0:63], op0=ALU.mult, op1=ALU.add), [t])
        else:
            t = B.c("dve", lambda e, wA=wA: e.scalar_tensor_tensor(
                out=acc[:, 64:TOWN], in0=u[:, 0:TOWN - 64], scalar=wA, in1=acc[:, 64:TOWN], op0=ALU.mult,
                op1=ALU.add), [t])
            t = B.c("dve", lambda e, wB=wB: e.scalar_tensor_tensor(
                out=acc[:, 0:TOWN - 64], in0=u[:, 64:TOWN], scalar=wB, in1=acc[:, 0:TOWN - 64], op0=ALU.mult,
                op1=ALU.add), [t])
            t = B.c("dve", lambda e, bf=bf: e.tensor_tensor(out=ub[:], in0=bf["bc"][:], in1=bf["bx"][:], op=ALU.mult),
                    [t])
            t = B.c("dve", lambda e, wA=wA: e.scalar_tensor_tensor(
                out=acc[:, 0:64], in0=ub[:], scalar=wA, in1=acc[:, 0:64], op0=ALU.mult, op1=ALU.add), [t])
        yi, yw = yring.acquire()
        ybf = yring.bufs[yi]
        t = B.c("dve", lambda e, bf=bf, ybf=ybf: e.tensor_tensor(out=ybf[:], in0=acc[:], in1=bf["cb"][:], op=ALU.mult),
                [t] + yw)
        tprev = t
        cring.release(slot, t)
        yring.store(yi, "sp", mixT_d[2048 + cb * 128:2048 + (cb + 1) * 128, :], ybf[:], [t])
    yring.drain("sp")
    B.run()

    B = Builder(nc, "p4")
    mixT = B.sb("mixT", [128, NCH, 1024], BF16)
    wbufs = [B.sb(f"w{i}", [128, NCH, 512], BF16) for i in range(3)]
    g1 = B.sb("g1", [128, D], F32)
    xsb = [B.sb(f"xs{i}", [128, 512], F32) for i in range(4)]
    o32 = [B.sb(f"o32{i}", [128, 512], F32) for i in range(4)]
    wring = DmaRing(B, "wr", wbufs)
    xsring = DmaRing(B, "xs", xsb)
    oring = StoreRing(B, "or", o32)
    lsem = B.sem("ld")
    g1t = B.dma("sp", g1[:], ada_d[0:1, 2 * D:3 * D].partition_broadcast(128), lsem)
    gbanks = PsBanks([0, 1, 2, 3, 4, 5, 6, 7])
    msem = B.sem("mix")
    for pi in range(2):
        lo = pi * 1024
        mw = [(B.prog["pe"], B.prog["pe"].cnt)]
        mt = None
        for q4 in range(4):
            mt = B.dma("sp", mixT[:, q4 * 8:(q4 + 1) * 8, :],
                       mixT_d[q4 * 1024:(q4 + 1) * 1024, lo:lo + 1024].rearrange("(c p) t -> p c t", p=128),
                       msem, mw)
        blocks = []
        for j in range(8):
            def part(wb, emit_group, j=j, lo=lo):
                for tt in range(8):
                    def mm(c, b, tt=tt):
                        return ps[:, b, 0:512], mixT[:, c, tt * 128:(tt + 1) * 128], wb[:, c, :]

                    def handler(b, pt, tt=tt):
                        r0 = lo + tt * 128
                        xi, xt = xsring.load("sp", lambda buf: [(buf[:], x_own[r0:r0 + 128, j * 512:(j + 1) * 512],
                                                                 {})])
                        xb = xsring.bufs[xi]
                        oi, ow = oring.acquire()
                        ob = oring.bufs[oi]
                        et = B.c("dve", lambda e: e.tensor_tensor(out=ob[:], in0=ps[:, b, 0:512],
                                                                 in1=g1[:, j * 512:(j + 1) * 512], op=ALU.mult),
                                 [pt, g1t] + ow)
                        e2 = B.c("pool", lambda e: e.tensor_tensor(out=ob[:], in0=ob[:], in1=xb[:], op=ALU.add),
                                 [et, xt])
                        xsring.release(xi, e2)
                        oring.store(oi, "sp", x1_d[r0:r0 + 128, j * 512:(j + 1) * 512], ob[:], [e2])
                        return et

                    emit_group(1, NCH, mm, handler)
            blocks.append(dict(dmas=lambda buf, j=j: [(buf[:], wslice(w_out, j * 512, 512), {})], parts=[part]))
        gemm(B, ps, gbanks, wring, blocks, [mt])
    oring.drain("sp")
    B.run()

    B = Builder(nc, "p5")
    ident, ident_tok = make_consts(B)
    vecs = B.sb("vecs", [128, 2, NCH], F32)
    xfT = B.sb("xfT", [128, NCH, 512], BF16)
    yacc = B.sb("yacc", [128, 4, D], F32)
    xf32 = yacc[:, 0, :].rearrange("p (c t) -> p c t", t=128)
    wbufs = [B.sb(f"w{i}", [128, NCH, 256], BF16) for i in range(2)]
    w2bufs = [B.sb(f"w2{i}", [128, 8, 512], BF16) for i in range(2)]
    actT = [B.sb(f"actT{i}", [128, 8, 512], BF16) for i in range(2)]
    sqj = actT[0][:].rearrange("p a b -> p (a b)")
    bigA = B.sb("bigA", [128, D], F32)
    bigB = B.sb("bigB", [128, D], F32)
    small = B.sb("small", [128, 8], F32)
    wr32 = B.sb("wr32", [128, NCH, 20], F32)
    br = B.sb("br", [128, 20], F32)
    gates = B.sb("gates", [128, 4, 16], F32)
    rt = B.sb("rt", [128, 64], F32)
    sa = [B.sb(f"sa{i}", [128, 512], F32) for i in range(2)]
    xring = DmaRing(B, "xr", [bigA, bigB])
    wring = DmaRing(B, "wr", wbufs)
    w2ring = DmaRing(B, "w2r", w2bufs)
    lsem = B.sem("ld")
    vtok = B.dma("sp", vecs[:, 0, :], vec_d[4], lsem)
    vtok = B.dma("sp", vecs[:, 1, :], vec_d[5], lsem)
    vtok = B.dma("sp", wr32[:], w_r.rearrange("(c p) n -> p c n", p=128), lsem)
    vtok = B.dma("sp", br[:], b_r.partition_broadcast(128), lsem)
    fbanks = PsBanks([0, 1])
    abanks = PsBanks([0, 1, 2, 3])
    ybanks = PsBanks([4, 5, 6, 7])
    osem = B.sem("out")
    bsem = B.sem("bc")
    out_tok = None
    blk_done = []

    for tb in range(4):
        t0 = tb * 512
        rstate = {"ps2_free": None, "final": [], "dve_last": None}

        def post_tile(i, etoks, tb=tb, rstate=rstate):
            tcst = B.c("pool", lambda e, i=i: e.tensor_copy(out=xfT[:, :, i * 128:(i + 1) * 128], in_=xf32),
                       list(etoks))
            w0 = list(etoks) + [vtok] + ([rstate["ps2_free"]] if rstate["ps2_free"] else [])
            tr = None
            for c in range(NCH):
                fn = lambda e, c=c: e.matmul(ps[:, 2, 0:20], lhsT=xf32[:, c, :], rhs=wr32[:, c, :], start=(c == 0),
                                             stop=(c == NCH - 1))
                if c == NCH - 1:
                    tr = B.c("pe", fn)
                else:
                    B.op("pe", fn, w0 if c == 0 else ())
            lg = rt[:, 0:20]

            def dv(fn, w):
                return B.c("dve", fn, w)

            t = dv(lambda e: e.tensor_tensor(out=lg, in0=ps[:, 2, 0:20], in1=br[:], op=ALU.add),
                   [tr, vtok] + ([rstate["dve_last"]] if rstate["dve_last"] else []))
            rstate["ps2_free"] = t
            mg = rt[:, 20:21]
            t = dv(lambda e: e.tensor_reduce(out=mg, in_=rt[:, 0:4], axis=mybir.AxisListType.X, op=ALU.max), [t])
            ohg = rt[:, 21:25]
            t = dv(lambda e: e.tensor_scalar(out=ohg, in0=rt[:, 0:4], scalar1=mg, scalar2=None, op0=ALU.is_equal), [t])
            eg4 = rt[:, 25:29]
            t = dv(lambda e: e.tensor_scalar(out=eg4, in0=rt[:, 0:4], scalar1=mg, scalar2=None, op0=ALU.subtract), [t])
            ta = B.c("act", lambda e: e.activation(out=eg4, in_=eg4, func=AF.Exp), [t])
            sg = rt[:, 29:30]
            t = dv(lambda e: e.tensor_reduce(out=sg, in_=eg4, axis=mybir.AxisListType.X, op=ALU.add), [ta])
            psel = rt[:, 30:31]
            t = dv(lambda e: e.reciprocal(out=psel, in_=sg), [t])
            egr = rt[:, 31:35]
            t = dv(lambda e: e.tensor_scalar(out=egr, in0=rt[:, 4:8], scalar1=rt[:, 21:22], scalar2=None,
                                             op0=ALU.mult), [t])
            for g in range(1, 4):
                t = dv(lambda e, g=g: e.scalar_tensor_tensor(out=egr, in0=rt[:, 4 + 4 * g:8 + 4 * g],
                                                             scalar=rt[:, 21 + g:22 + g], in1=egr, op0=ALU.mult,
                                                             op1=ALU.add), [t])
            m1 = rt[:, 35:36]
            t = dv(lambda e: e.tensor_reduce(out=m1, in_=egr, axis=mybir.AxisListType.X, op=ALU.max), [t])
            oh1 = rt[:, 36:40]
            t = dv(lambda e: e.tensor_scalar(out=oh1, in0=egr, scalar1=m1, scalar2=None, op0=ALU.is_equal), [t])
            msk = rt[:, 40:44]
            t = dv(lambda e: e.scalar_tensor_tensor(out=msk, in0=oh1, scalar=-1e30, in1=egr, op0=ALU.mult,
                                                    op1=ALU.add), [t])
            m2 = rt[:, 44:45]
            t = dv(lambda e: e.tensor_reduce(out=m2, in_=msk, axis=mybir.AxisListType.X, op=ALU.max), [t])
            oh2 = rt[:, 45:49]
            t = dv(lambda e: e.tensor_scalar(out=oh2, in0=msk, scalar1=m2, scalar2=None, op0=ALU.is_equal), [t])
            dd = rt[:, 49:50]
            t = dv(lambda e: e.tensor_tensor(out=dd, in0=m2, in1=m1, op=ALU.subtract), [t])
            ta = B.c("act", lambda e: e.activation(out=dd, in_=dd, func=AF.Exp), [t])
            w1s = rt[:, 50:51]
            t = dv(lambda e: e.tensor_scalar(out=w1s, in0=dd, scalar1=1.0, scalar2=None, op0=ALU.add), [ta])
            t = dv(lambda e: e.reciprocal(out=w1s, in_=w1s), [t])
            w2s = rt[:, 51:52]
            t = dv(lambda e: e.tensor_tensor(out=w2s, in0=dd, in1=w1s, op=ALU.mult), [t])
            t = dv(lambda e: e.tensor_tensor(out=w1s, in0=w1s, in1=psel, op=ALU.mult), [t])
            t = dv(lambda e: e.tensor_tensor(out=w2s, in0=w2s, in1=psel, op=ALU.mult), [t])
            gg = rt[:, 52:56]
            t = dv(lambda e: e.tensor_scalar(out=gg, in0=oh1, scalar1=w1s, scalar2=None, op0=ALU.mult), [t])
            t = dv(lambda e: e.scalar_tensor_tensor(out=gg, in0=oh2, scalar=w2s, in1=gg, op0=ALU.mult, op1=ALU.add),
                   [t])
            for g in range(4):
                t = dv(lambda e, g=g, i=i: e.tensor_scalar(out=gates[:, i, 4 * g:4 * g + 4], in0=gg,
                                                           scalar1=rt[:, 21 + g:22 + g], scalar2=None, op0=ALU.mult),
                       [t])
            rstate["dve_last"] = t
            rstate["final"] = [t, tcst, tr]
            for e_ in ("act", "dve"):
                B.wait(e_, tcst)
                B.wait(e_, tr)

        ftoks = fill_tiles(B, ps, fbanks, ident, ident_tok, xring,
                           lambda i, t0=t0: x1_d[t0 + i * 128:t0 + (i + 1) * 128, :], 4, vecs[:, 0, :], vecs[:, 1, :],
                           lambda c, i: xf32[:, c, :], sqj, small, post_tile=post_tile,
                           extra_first=[vtok] + blk_done)
        fill_done = list(rstate["final"])
        w2pending = None
        acnt = [0]
        sa_free = [None, None]
        for ex in range(16):
            at = actT[ex % 2]
            blocks = []
            act_toks = []
            for i in range(8):
                def part(wb, emit_group, i=i, at=at):
                    res = {}
                    for half in range(2):
                        def mm(c, b, half=half):
                            return ps[:, b, 0:512], wb[:, c, half * 128:(half + 1) * 128], xfT[:, c, :]

                        def handler(b, pt, half=half):
                            if half == 0:
                                si = acnt[0] % 2
                                sab = sa[si]
                                acnt[0] += 1
                                res["sab"] = sab
                                res["si"] = si
                                et = B.c("act", lambda e: e.activation(out=sab[:], in_=ps[:, b, 0:512], func=AF.Silu),
                                         [pt] + ([sa_free[si]] if sa_free[si] else []))
                                res["sat"] = et
                                return et
                            sab = res["sab"]
                            et = B.c("dve", lambda e: e.tensor_tensor(out=at[:, i, :], in0=ps[:, b, 0:512],
                                                                     in1=sab[:], op=ALU.mult), [pt, res["sat"]])
                            act_toks.append(et)
                            sa_free[res["si"]] = et
                            return et

                        emit_group(1, NCH, mm, handler)
                blocks.append(dict(dmas=lambda buf, ex=ex, i=i: [(buf[:], wslice(w_e1[ex], i * 256, 256), {})],
                                   parts=[part]))
            gemm(B, ps, abanks, wring, blocks, fill_done)
            blocks2 = []
            for j in range(8):
                def part2(wb, emit_group, j=j, ex=ex, at=at):
                    for tt in range(4):
                        def mm(c, b, tt=tt):
                            return ps[:, b, 0:512], at[:, c, tt * 128:(tt + 1) * 128], wb[:, c, :]

                        def handler(b, pt, tt=tt):
                            gsc = gates[:, tt, ex:ex + 1]
                            ya = yacc[:, tt, j * 512:(j + 1) * 512]
                            if ex == 0:
                                return B.c("dve", lambda e: e.tensor_scalar(out=ya, in0=ps[:, b, 0:512], scalar1=gsc,
                                                                           scalar2=None, op0=ALU.mult),
                                           [pt] + fill_done)
                            return B.c("dve", lambda e: e.scalar_tensor_tensor(out=ya, in0=ps[:, b, 0:512],
                                                                              scalar=gsc, in1=ya, op0=ALU.mult,
                                                                              op1=ALU.add), [pt])

                        emit_group(1, 8, mm, handler)
                blocks2.append(dict(dmas=lambda buf, ex=ex, j=j: [
                    (buf[:], w_e2[ex][:, j * 512:(j + 1) * 512].rearrange("(c p) n -> p c n", p=128), {})],
                    parts=[part2]))
            gemm(B, ps, ybanks, w2ring, blocks2, [act_toks[-1]])
            at_free = (B.prog["pe"], B.prog["pe"].cnt)
            B.wait("dve", at_free)
        ydone = (B.prog["dve"], B.prog["dve"].cnt)
        pe_done = (B.prog["pe"], B.prog["pe"].cnt)
        g2t = B.dma("sp", bigA[:], ada_d[0:1, 5 * D:6 * D].partition_broadcast(128), bsem, [pe_done, ydone])
        tz = None
        xtoks = []
        zt = []
        for tt in range(4):
            r0 = t0 + tt * 128
            xt_ = B.dma("sp", bigB[:], x1_d[r0:r0 + 128, :], bsem, [pe_done, ydone] + zt[-1:])
            t = B.c("dve", lambda e, tt=tt: e.tensor_tensor(out=yacc[:, tt, :], in0=yacc[:, tt, :], in1=bigA[:],
                                                           op=ALU.mult), [g2t, ydone])
            t = B.c("pool", lambda e, tt=tt: e.tensor_tensor(out=yacc[:, tt, :], in0=yacc[:, tt, :], in1=bigB[:],
                                                            op=ALU.add), [t, xt_])
            zt.append(t)
        fnt = B.dma("sp", bigA[:], final_norm.partition_broadcast(128), bsem, [(B.prog["dve"], B.prog["dve"].cnt)])
        for tt in range(4):
            r0 = t0 + tt * 128
            ss = small[:, 0:1]
            rs = small[:, 1:2]
            t = B.c("act", lambda e, tt=tt: e.activation(out=bigB[:], in_=yacc[:, tt, :], func=AF.Square,
                                                       accum_out=ss), [zt[tt], zt[3]])
            t = B.c("act", lambda e: e.activation(out=ss, in_=ss, func=AF.Sqrt, scale=1.0 / D, bias=EPS), [t])
            t = B.c("dve", lambda e: e.reciprocal(out=rs, in_=ss), [t])
            t = B.c("dve", lambda e, tt=tt: e.scalar_tensor_tensor(out=yacc[:, tt, :], in0=yacc[:, tt, :], scalar=rs,
                                                                  in1=bigA[:], op0=ALU.mult, op1=ALU.mult), [t, fnt])
            B.wait("act", t)
            out_tok = B.dma("sp", out[r0:r0 + 128, :], yacc[:, tt, :], osem, [t])
        blk_done = [out_tok, (B.prog["dve"], B.prog["dve"].cnt), (B.prog["act"], B.prog["act"].cnt),
                    (B.prog["pool"], B.prog["pool"].cnt)]
    B.wait("sp", out_tok)
    B.run()
    return nc


_CACHE = {}


def kernel(x, c, ctx, c_ctx, w_ada, b_ada, norm1, w_in, b_gates, conv_w, head_norm, w_out, norm2,
           w_router_group, b_router_group, w_router_expert, b_router_expert,
           w_expert_in, w_expert_out, final_norm):
    f = np.float32
    x = np.asarray(x, f)
    ctx = np.asarray(ctx, f)
    c = np.asarray(c, f)
    c_ctx = np.asarray(c_ctx, f)
    w_in0 = np.ascontiguousarray(np.asarray(w_in, f)[0])
    w_ada0 = np.ascontiguousarray(np.asarray(w_ada, f)[0])
    b_ada0 = np.ascontiguousarray(np.asarray(b_ada, f)[0]).reshape(1, -1)
    w_out0 = np.ascontiguousarray(np.asarray(w_out, f)[0])
    normsT = np.ascontiguousarray(
        np.stack([np.asarray(norm1, f)[0], np.asarray(norm2, f)[0]]).reshape(2, NCH, 128).transpose(2, 0, 1))
    wg_nat = np.ascontiguousarray(w_in0[:, 12288:12320])
    perm = np.concatenate([np.arange(16, 32), np.arange(0, 16)])
    wg_sw = np.ascontiguousarray(wg_nat[:, perm])
    bg_nat = np.asarray(b_gates, f)[0].reshape(1, 32)
    bg_sw = np.ascontiguousarray(bg_nat[:, perm])
    cw = np.asarray(conv_w, f)[0]
    cw_nat = np.ascontiguousarray(cw.reshape(3, 16, 128).transpose(2, 0, 1))
    cw_sw = np.ascontiguousarray(cw[::-1].reshape(3, 16, 128).transpose(2, 0, 1))
    hn = np.asarray(head_norm, f)[0].reshape(1, 2048)
    w_r = np.ascontiguousarray(np.concatenate([np.asarray(w_router_group, f)[0], np.asarray(w_router_expert, f)[0]], 1))
    b_r = np.concatenate([np.asarray(b_router_group, f)[0], np.asarray(b_router_expert, f)[0]]).reshape(1, 20)
    we1 = np.asarray(w_expert_in, f)[0]
    we1 = np.ascontiguousarray(we1.reshape(16, D, 2, 8, 128).transpose(0, 1, 3, 2, 4).reshape(16, D, 2048))
    we2 = np.ascontiguousarray(np.asarray(w_expert_out, f)[0])
    fn = np.asarray(final_norm, f).reshape(1, D)

    in_maps = []
    for core in range(8):
        b, hf = core // 2, core % 2
        if hf == 1:
            xo = x[b, 2048:]
            oth = x[b, :2048]
            cp = ctx[b]
            wg, bg, cwt = wg_nat, bg_nat, cw_nat
        else:
            xo = x[b, :2048][::-1]
            oth = x[b, 2048:][::-1]
            cp = ctx[b][::-1]
            wg, bg, cwt = wg_sw, bg_sw, cw_sw
        cT = np.ascontiguousarray(np.stack([c[b], c_ctx]).reshape(2, NCH, 128).transpose(2, 1, 0))
        in_maps.append({
            "x_own": np.ascontiguousarray(xo), "x_oc": np.ascontiguousarray(np.concatenate([cp, oth], 0)),
            "cT": cT, "w_ada": w_ada0, "b_ada": b_ada0, "normsT": normsT, "w_in": w_in0, "w_g": wg, "b_g": bg,
            "conv_wT": cwt, "head_norm": hn, "w_out": w_out0, "w_r": w_r, "b_r": b_r, "w_e1": we1, "w_e2": we2,
            "final_norm": fn,
        })
    if "nc" not in _CACHE:
        _CACHE["nc"] = build_program()
    res = run_bass_kernel_spmd(_CACHE["nc"], in_maps, core_ids=list(range(8)))
    if _DEBUG:
        _CACHE["res"] = res.results
    outp = np.empty((4, 4096, D), f)
    for core in range(8):
        b, hf = core // 2, core % 2
        o = res.results[core]["out"]
        if hf == 1:
            outp[b, 2048:] = o
        else:
            outp[b, :2048] = o[::-1]
    return outp
```
